# Optimizing a Trainium2 kernel written in Bass

```python
import jax
import jax.numpy as jnp
from jax import lax
import numpy as np

D_MODEL = 1024
BATCH = 4
SEQ = 8192
DEPTH = 4

D_MIX = D_MODEL
GROUP_WIDTH = D_MIX // 4
MLA_V_DIM = 64
MLA_HEADS = GROUP_WIDTH // MLA_V_DIM
MLA_NOPE_DIM = 64
MLA_ROPE_DIM = 32
MLA_QK_DIM = MLA_NOPE_DIM + MLA_ROPE_DIM
MLA_Q_LORA = 3 * D_MODEL // 16
MLA_KV_LORA = D_MODEL // 8
ROPE_THETA = 10000.0
ATTN_BLOCK = 128
POOL_WINDOWS = (2, 4, 8, 16)
POOL_GROUP = GROUP_WIDTH // len(POOL_WINDOWS)
HGRN_DK = 64
HGRN_DV = 64
HGRN_HEADS = GROUP_WIDTH // HGRN_DV
HGRN_CHUNK = 128
CONV_CH = GROUP_WIDTH
CONV_K = 31
N_EXPERT_GROUPS = 4
EXPERTS_PER_GROUP = 8
N_EXPERTS = N_EXPERT_GROUPS * EXPERTS_PER_GROUP
TOP_K = 2
D_EXPERT = D_MODEL // 2
MOE_BLOCK = 128
EPS = 1e-6
IN_SIZES = (MLA_Q_LORA, MLA_KV_LORA, MLA_ROPE_DIM, GROUP_WIDTH,
            HGRN_HEADS * HGRN_DK, HGRN_HEADS * HGRN_DK, HGRN_HEADS * HGRN_DV, HGRN_HEADS * HGRN_DV,
            2 * CONV_CH)
IN_COLS = sum(IN_SIZES)
IN_OFFSETS = tuple(int(v) for v in np.cumsum(IN_SIZES)[:-1])

kernel_name = 'hybrid_parallel_heads_hmoe'


def rms_norm(x, g):
    xf = x.astype(jnp.float32)
    y = xf * lax.rsqrt(jnp.mean(xf * xf, axis=-1, keepdims=True) + EPS)
    return (y * g.astype(jnp.float32)).astype(x.dtype)


def layer_norm(x, g, b):
    xf = x.astype(jnp.float32)
    mu = jnp.mean(xf, axis=-1, keepdims=True)
    var = jnp.mean(jnp.square(xf - mu), axis=-1, keepdims=True)
    y = (xf - mu) * lax.rsqrt(var + EPS)
    return (y * g.astype(jnp.float32) + b.astype(jnp.float32)).astype(x.dtype)


def rope_tables(positions):
    half = MLA_ROPE_DIM // 2
    inv_freq = ROPE_THETA ** (-jnp.arange(half, dtype=jnp.float32) / half)
    ang = positions.astype(jnp.float32)[..., None] * inv_freq
    return jnp.cos(ang)[:, :, None, :], jnp.sin(ang)[:, :, None, :]


def apply_rope(x, cos, sin):
    x1, x2 = jnp.split(x, 2, axis=-1)
    cos = cos.astype(x.dtype)
    sin = sin.astype(x.dtype)
    return jnp.concatenate([x1 * cos - x2 * sin, x1 * sin + x2 * cos], axis=-1)


def causal_attention(q, k, v):
    B, S, H, Dq = q.shape
    nb = S // ATTN_BLOCK
    qb = q.reshape(B, nb, ATTN_BLOCK, H, Dq).transpose(1, 0, 2, 3, 4)
    starts = jnp.arange(nb, dtype=jnp.int32) * ATTN_BLOCK
    kpos = jnp.arange(S, dtype=jnp.int32)
    scale = MLA_QK_DIM ** -0.5

    def block(args):
        qi, st = args
        s = jnp.einsum('bqhd,bkhd->bhqk', qi, k).astype(jnp.float32) * scale
        qpos = st + jnp.arange(ATTN_BLOCK, dtype=jnp.int32)
        s = jnp.where(kpos[None, :] <= qpos[:, None], s, -1e30)
        p = jax.nn.softmax(s, axis=-1).astype(v.dtype)
        return jnp.einsum('bhqk,bkhd->bqhd', p, v)

    o = lax.map(block, (qb, starts))
    return o.transpose(1, 0, 2, 3, 4).reshape(B, S, H, v.shape[-1])


def mla_mixer(cq, ckv, krope, cos, sin, q_a_g, w_uq, kv_a_g, w_ukv, q_g, k_g):
    B, S, _ = cq.shape
    q = (rms_norm(cq, q_a_g) @ w_uq).reshape(B, S, MLA_HEADS, MLA_QK_DIM)
    kv = (rms_norm(ckv, kv_a_g) @ w_ukv).reshape(B, S, MLA_HEADS, MLA_NOPE_DIM + MLA_V_DIM)
    k_nope, v = jnp.split(kv, [MLA_NOPE_DIM], axis=-1)
    k_rope = jnp.broadcast_to(krope[:, :, None, :], (B, S, MLA_HEADS, MLA_ROPE_DIM))
    k = jnp.concatenate([k_nope, k_rope], axis=-1)
    q = rms_norm(q, q_g)
    k = rms_norm(k, k_g)
    q = jnp.concatenate([q[..., :MLA_NOPE_DIM], apply_rope(q[..., MLA_NOPE_DIM:], cos, sin)], axis=-1)
    k = jnp.concatenate([k[..., :MLA_NOPE_DIM], apply_rope(k[..., MLA_NOPE_DIM:], cos, sin)], axis=-1)
    o = causal_attention(q, k, v)
    return o.reshape(B, S, MLA_HEADS * MLA_V_DIM)


def pool_mixer(u, w_pool, scale):
    B, S, _ = u.shape
    uf = u.astype(jnp.float32).reshape(B, S, len(POOL_WINDOWS), POOL_GROUP)
    cs = jnp.cumsum(uf, axis=1)
    t1 = jnp.arange(1, S + 1, dtype=jnp.float32)
    outs = []
    for gi, w in enumerate(POOL_WINDOWS):
        csg = cs[:, :, gi]
        lag = jnp.pad(csg, ((0, 0), (w, 0), (0, 0)))[:, :S]
        mean = (csg - lag) / jnp.minimum(t1, w)[None, :, None]
        outs.append(mean - uf[:, :, gi])
    pooled = jnp.stack(outs, axis=2).astype(u.dtype)
    y = jnp.einsum('bsgc,gcd->bsgd', pooled, w_pool)
    return y.reshape(B, S, GROUP_WIDTH) * scale


def gated_recurrence_chunked(q, k, v, logf):
    B, S, H, DK = q.shape
    DV = v.shape[-1]
    nc = S // HGRN_CHUNK

    def to_chunks(a):
        return a.reshape(B, nc, HGRN_CHUNK, H, a.shape[-1]).transpose(1, 0, 3, 2, 4)

    causal = jnp.tril(jnp.ones((HGRN_CHUNK, HGRN_CHUNK), dtype=bool))

    def step(state, inp):
        qc, kc, vc, lc = inp
        b = jnp.cumsum(lc, axis=2)
        o_inter = jnp.einsum('bhtd,bhdv->bhtv', qc * jnp.exp(b), state)
        diff = b[:, :, :, None, :] - b[:, :, None, :, :]
        decay = jnp.exp(jnp.where(causal[:, :, None], diff, -jnp.inf))
        a = jnp.einsum('bhtd,bhtsd,bhsd->bhts', qc, decay, kc)
        o_intra = jnp.einsum('bhts,bhsv->bhtv', a, vc)
        b_last = b[:, :, -1:, :]
        new_state = jnp.exp(b_last[:, :, 0, :])[..., None] * state + \
            jnp.einsum('bhsd,bhsv->bhdv', kc * jnp.exp(b_last - b), vc)
        return new_state, o_inter + o_intra

    state0 = jnp.zeros((B, H, DK, DV), jnp.float32)
    _, o = lax.scan(step, state0, (to_chunks(q), to_chunks(k), to_chunks(v), to_chunks(logf)))
    return o.transpose(1, 0, 3, 2, 4).reshape(B, S, H, DV)


def hgrn2_mixer(hq, hf, hi, hg, lb, out_g):
    B, S, _ = hq.shape
    q = jax.nn.silu(hq.astype(jnp.float32)).reshape(B, S, HGRN_HEADS, HGRN_DK)
    zf = hf.astype(jnp.float32).reshape(B, S, HGRN_HEADS, HGRN_DK)
    lbh = lb.reshape(HGRN_HEADS, HGRN_DK)
    logf = jnp.logaddexp(jnp.log(lbh), jnp.log1p(-lbh) + jax.nn.log_sigmoid(zf))
    k = -jnp.expm1(logf)
    v = hi.astype(jnp.float32).reshape(B, S, HGRN_HEADS, HGRN_DV)
    o = gated_recurrence_chunked(q, k, v, logf)
    g = hg.astype(jnp.float32).reshape(B, S, HGRN_HEADS, HGRN_DV)
    o = rms_norm(o, out_g) * jax.nn.silu(g)
    return o.reshape(B, S, GROUP_WIDTH).astype(hq.dtype)


def conformer_conv_mixer(u2, dw_w, dw_b, ln_g, ln_b):
    a, gate = jnp.split(u2, 2, axis=-1)
    u = a * jax.nn.sigmoid(gate)
    u = lax.conv_general_dilated(
        u, dw_w[:, None, :].astype(u.dtype), window_strides=(1,), padding=[(CONV_K - 1, 0)],
        dimension_numbers=('NWC', 'WIO', 'NWC'), feature_group_count=CONV_CH) + dw_b
    u = layer_norm(u, ln_g, ln_b)
    return jax.nn.silu(u)


def token_mixer(h, cos, sin, lb, w_in, q_a_g, w_uq, kv_a_g, w_ukv, q_g, k_g, w_pool, pool_scale,
                hgrn_out_g, dw_w, dw_b, ln_g, ln_b, w_out):
    z = h @ w_in
    cq, ckv, krope, u_pool, hq, hf, hi, hg, u_conv = jnp.split(z, list(IN_OFFSETS), axis=-1)
    y_mla = mla_mixer(cq, ckv, krope, cos, sin, q_a_g, w_uq, kv_a_g, w_ukv, q_g, k_g)
    y_pool = pool_mixer(u_pool, w_pool, pool_scale)
    y_hgrn = hgrn2_mixer(hq, hf, hi, hg, lb, hgrn_out_g)
    y_conv = conformer_conv_mixer(u_conv, dw_w, dw_b, ln_g, ln_b)
    y = jnp.concatenate([y_mla, y_pool, y_hgrn, y_conv], axis=-1)
    return y @ w_out


def hierarchical_moe(h, rg_w, rg_b, re_w, re_b, w1, w3, w2):
    B, S, D = h.shape
    N = B * S
    xf = h.reshape(N, D)
    g_logits = (xf @ rg_w).astype(jnp.float32) + rg_b
    g_prob = jax.nn.softmax(g_logits, axis=-1)
    g_idx = jnp.argmax(g_logits, axis=-1).astype(jnp.int32)
    g_w = jnp.take_along_axis(g_prob, g_idx[:, None], axis=-1)
    e_logits = ((xf @ re_w).astype(jnp.float32) + re_b).reshape(N, N_EXPERT_GROUPS, EXPERTS_PER_GROUP)
    e_logits = jnp.take_along_axis(e_logits, g_idx[:, None, None], axis=1)[:, 0]
    top_l, top_i = lax.top_k(e_logits, TOP_K)
    e_w = jax.nn.softmax(top_l, axis=-1) * g_w
    expert_id = (g_idx[:, None] * EXPERTS_PER_GROUP + top_i).reshape(-1).astype(jnp.int32)
    weight = e_w.reshape(-1)
    token = jnp.repeat(jnp.arange(N, dtype=jnp.int32), TOP_K)
    A = N * TOP_K
    order = jnp.argsort(expert_id)
    e_sorted = expert_id[order]
    tok_sorted = token[order]
    w_sorted = weight[order]
    counts = jax.ops.segment_sum(jnp.ones((A,), jnp.int32), expert_id, num_segments=N_EXPERTS)
    start = jnp.cumsum(counts) - counts
    padded = (counts + MOE_BLOCK - 1) // MOE_BLOCK * MOE_BLOCK
    padded_end = jnp.cumsum(padded)
    padded_start = padded_end - padded
    dest = padded_start[e_sorted] + jnp.arange(A, dtype=jnp.int32) - start[e_sorted]
    n_rows = (-(-A // MOE_BLOCK) + N_EXPERTS) * MOE_BLOCK
    n_blocks = n_rows // MOE_BLOCK
    row_tok = jnp.full((n_rows,), N, jnp.int32).at[dest].set(tok_sorted)
    row_w = jnp.zeros((n_rows,), jnp.float32).at[dest].set(w_sorted)
    blk_start = jnp.arange(n_blocks, dtype=jnp.int32) * MOE_BLOCK
    blk_expert = jnp.minimum(jnp.searchsorted(padded_end, blk_start, side='right'), N_EXPERTS - 1)
    x_pad = jnp.concatenate([xf, jnp.zeros((1, D), xf.dtype)], axis=0)

    def run_block(args):
        toks, wts, e = args
        xb = x_pad[toks]
        hb = jax.nn.silu(xb @ w1[e]) * (xb @ w3[e])
        return (hb @ w2[e]) * wts[:, None].astype(xb.dtype)

    yb = lax.map(run_block, (row_tok.reshape(n_blocks, MOE_BLOCK), row_w.reshape(n_blocks, MOE_BLOCK), blk_expert))
    y = jnp.zeros((N + 1, D), h.dtype).at[row_tok].add(yb.reshape(n_rows, D).astype(h.dtype))
    return y[:N].reshape(B, S, D)


def setup_inputs(seed: int = 0) -> dict:
    key = jax.random.key(seed)
    ks = jax.random.split(key, 32)
    f32 = jnp.float32
    L, D = DEPTH, D_MODEL

    def nrm(k, shape, scale):
        return jax.random.normal(k, shape, f32) * scale

    def gain(k, shape):
        return 1.0 + 0.02 * jax.random.normal(k, shape, f32)

    return {
        'x': nrm(ks[0], (BATCH, SEQ, D), 1.0),
        'c': nrm(ks[1], (BATCH, D), 1.0),
        'positions': jnp.arange(SEQ, dtype=jnp.int32)[None, :] + jax.random.randint(ks[2], (BATCH, 1), 0, 4096, dtype=jnp.int32),
        'w_ada': nrm(ks[3], (L, D, 6 * D), 0.5 * D ** -0.5),
        'b_ada': nrm(ks[4], (L, 6 * D), 0.02),
        'norm1_g': gain(ks[5], (L, D)),
        'norm2_g': gain(ks[6], (L, D)),
        'w_in': nrm(ks[7], (L, D, IN_COLS), D ** -0.5),
        'q_a_norm_g': gain(ks[8], (L, MLA_Q_LORA)),
        'w_uq': nrm(ks[9], (L, MLA_Q_LORA, MLA_HEADS * MLA_QK_DIM), MLA_Q_LORA ** -0.5),
        'kv_a_norm_g': gain(ks[10], (L, MLA_KV_LORA)),
        'w_ukv': nrm(ks[11], (L, MLA_KV_LORA, MLA_HEADS * (MLA_NOPE_DIM + MLA_V_DIM)), MLA_KV_LORA ** -0.5),
        'q_norm_g': gain(ks[12], (L, MLA_QK_DIM)),
        'k_norm_g': gain(ks[13], (L, MLA_QK_DIM)),
        'w_pool': nrm(ks[14], (L, len(POOL_WINDOWS), POOL_GROUP, POOL_GROUP), POOL_GROUP ** -0.5),
        'pool_scale': 1.0 + 0.1 * jax.random.normal(ks[15], (L, GROUP_WIDTH), f32),
        'hgrn_lb_logits': nrm(ks[16], (DEPTH, HGRN_HEADS * HGRN_DK), 1.0),
        'hgrn_out_norm_g': gain(ks[17], (L, HGRN_DV)),
        'conv_dw_w': nrm(ks[18], (L, CONV_K, CONV_CH), CONV_K ** -0.5),
        'conv_dw_b': nrm(ks[19], (L, CONV_CH), 0.02),
        'conv_ln_g': gain(ks[20], (L, CONV_CH)),
        'conv_ln_b': nrm(ks[21], (L, CONV_CH), 0.02),
        'w_out': nrm(ks[22], (L, D_MIX, D), D_MIX ** -0.5),
        'router_group_w': nrm(ks[23], (L, D, N_EXPERT_GROUPS), D ** -0.5),
        'router_group_b': nrm(ks[24], (L, N_EXPERT_GROUPS), 0.01),
        'router_expert_w': nrm(ks[25], (L, D, N_EXPERTS), D ** -0.5),
        'router_expert_b': nrm(ks[26], (L, N_EXPERTS), 0.01),
        'w1': nrm(ks[27], (L, N_EXPERTS, D, D_EXPERT), D ** -0.5),
        'w3': nrm(ks[28], (L, N_EXPERTS, D, D_EXPERT), D ** -0.5),
        'w2': nrm(ks[29], (L, N_EXPERTS, D_EXPERT, D), D_EXPERT ** -0.5),
    }


def reference(x, c, positions, w_ada, b_ada, norm1_g, norm2_g, w_in, q_a_norm_g, w_uq, kv_a_norm_g, w_ukv,
              q_norm_g, k_norm_g, w_pool, pool_scale, hgrn_lb_logits, hgrn_out_norm_g, conv_dw_w, conv_dw_b,
              conv_ln_g, conv_ln_b, w_out, router_group_w, router_group_b, router_expert_w, router_expert_b,
              w1, w3, w2):
    cos, sin = rope_tables(positions)
    lb_cum = jnp.cumsum(jax.nn.softmax(hgrn_lb_logits.astype(jnp.float32), axis=0), axis=0)
    lower_bounds = lb_cum - lb_cum[0:1]
    c_act = jax.nn.silu(c)
    for l in range(DEPTH):
        mod = (c_act @ w_ada[l] + b_ada[l])[:, None, :]
        sh1, sc1, g1, sh2, sc2, g2 = jnp.split(mod, 6, axis=-1)
        h = rms_norm(x, norm1_g[l]) * (1 + sc1) + sh1
        x = x + g1 * token_mixer(h, cos, sin, lower_bounds[l], w_in[l], q_a_norm_g[l], w_uq[l], kv_a_norm_g[l],
                                 w_ukv[l], q_norm_g[l], k_norm_g[l], w_pool[l], pool_scale[l], hgrn_out_norm_g[l],
                                 conv_dw_w[l], conv_dw_b[l], conv_ln_g[l], conv_ln_b[l], w_out[l])
        h = rms_norm(x, norm2_g[l]) * (1 + sc2) + sh2
        x = x + g2 * hierarchical_moe(h, router_group_w[l], router_group_b[l], router_expert_w[l],
                                      router_expert_b[l], w1[l], w3[l], w2[l])
    return x
```

```python
import os
import numpy as np
import concourse.bass as bass
import concourse.mybir as mybir
from concourse.bass_utils import run_bass_kernel_spmd

F32 = mybir.dt.float32
BF16 = mybir.dt.bfloat16
I32 = mybir.dt.int32
U32 = mybir.dt.uint32
AF = mybir.ActivationFunctionType
ALU = mybir.AluOpType
ds = bass.ds

D = 1024
KC = 8
BLK = 256
IN_COLS = 2144
NEXP = 32
DEXP = 512
EPS = 1e-6
STAGE = float(os.environ.get('KSTAGE', '99'))


class Tok:
    __slots__ = ("w", "r", "name", "excl")

    def __init__(self, name="", excl=False):
        self.w = {}
        self.r = {}
        self.name = name
        self.excl = excl


class FW:
    def __init__(self, nc):
        self.nc = nc
        self.eng = {"pe": nc.tensor, "act": nc.scalar, "dve": nc.vector, "pool": nc.gpsimd, "sp": nc.sync}
        self.sem = {e: nc.alloc_semaphore("sem_" + e) for e in ("pe", "act", "dve", "pool")}
        self.cnt = {e: 0 for e in self.sem}
        self.known = {e: {} for e in self.eng}
        self.rings = {}
        for q, n in (("sp", 24), ("pool", 12), ("act", 6)):
            self.rings[q] = {"i": 0, "slots": [[nc.alloc_semaphore("dq_%s%d" % (q, i)), 0] for i in range(n)]}
        self.banks = []
        self.held = set()
        self.bank_i = 0
        self.nins = 0

    def _wait(self, e, sem, val):
        k = self.known[e]
        key = id(sem)
        if k.get(key, 0) >= val:
            return
        self.eng[e].wait_ge(sem, val)
        k[key] = val

    def _deps(self, e, R, W, Wadd=(), is_dma=False):
        deps = []
        for t in R:
            deps.extend(t.w.values())
        for t in W:
            deps.extend(t.w.values())
        for t in Wadd:
            for st in t.w.values():
                if st[2] != "dma" and (st[2] != e or is_dma):
                    deps.append(st)
        for t in list(W) + list(Wadd):
            for src, st in t.r.items():
                if src == e and e == "pe":
                    continue
                deps.append(st)
        for st in deps:
            sem, val, src = st
            if src == e and e == "pe":
                continue
            self._wait(e, sem, val)

    def _stamp(self, st, R, W, Wadd, rkey):
        for t in R:
            t.r[rkey] = st
        for t in W:
            t.w = {id(st[0]): st}
            t.r = {}
        for t in Wadd:
            t.w[id(st[0])] = st

    def op(self, e, fn, R=(), W=(), Wadd=()):
        if e != "pe":
            ex = [t for t in R if t.excl]
            if ex:
                R = [t for t in R if not t.excl]
                W = list(W) + ex
        self._deps(e, R, W, Wadd)
        ins = fn(self.eng[e])
        self.cnt[e] += 1
        self.nins += 1
        ins.then_inc(self.sem[e], 1)
        st = (self.sem[e], self.cnt[e], e)
        self._stamp(st, R, W, Wadd, e)
        return ins

    def dma(self, q, out, in_, R=(), W=(), Wadd=(), fn=None, **kw):
        self._deps(q, R, W, Wadd, is_dma=True)
        ring = self.rings[q]
        slot = ring["slots"][ring["i"]]
        sid = ring["i"]
        ring["i"] = (ring["i"] + 1) % len(ring["slots"])
        if slot[1] > 0:
            self._wait(q, slot[0], 16 * slot[1])
        if fn is None:
            ins = self.eng[q].dma_start(out=out, in_=in_, **kw)
        else:
            ins = fn(self.eng[q])
        slot[1] += 1
        self.nins += 1
        ins.then_inc(slot[0], 16)
        st = (slot[0], 16 * slot[1], "dma")
        self._stamp(st, R, W, Wadd, ("dma", q, sid))
        return ins

    def sync(self, e, R=(), W=()):
        self._deps(e, R, W)

    def finish(self, toks):
        for t in toks:
            self._deps("sp", [t], [t])
        for q in self.rings:
            for slot in self.rings[q]["slots"]:
                if slot[1] > 0:
                    self._wait("sp", slot[0], 16 * slot[1])
        for e in self.sem:
            if self.cnt[e] > 0:
                self._wait("sp", self.sem[e], self.cnt[e])

    def init_banks(self):
        for i in range(8):
            t = self.nc.alloc_psum_tensor("pbank%d" % i, [128, 512], F32)
            self.banks.append((t, Tok("pb%d" % i, excl=True)))

    def bank(self, hold=False):
        for _ in range(8):
            i = self.bank_i
            self.bank_i = (self.bank_i + 1) % 8
            if i not in self.held:
                if hold:
                    self.held.add(i)
                return i, self.banks[i][0], self.banks[i][1]
        raise RuntimeError("no free psum bank")

    def release(self, i):
        self.held.discard(i)


def _r(ap, pattern, **kw):
    return ap.rearrange(pattern, **kw)


class Builder:
    def __init__(self, S, L, dbg=False):
        self.S = S
        self.L = L
        self.NB = S // BLK
        self.NT = S // 128
        self.dbg = dbg
        self.NBLKS = (S * 2) // 128 + NEXP
        self.NSLOT = self.NBLKS * 128
        nc = bass.Bass("TRN2", target_bir_lowering=False)
        self.nc = nc
        self.fw = FW(nc)
        self.fw.init_banks()
        self.sb_i = 0

    def sb(self, shape, dt, name=None):
        self.sb_i += 1
        return self.nc.alloc_sbuf_tensor("%s_%d" % (name or "t", self.sb_i), list(shape), dt)

    def din(self, name, shape, dt=F32):
        return self.nc.dram_tensor(name, list(shape), dt, kind="ExternalInput").ap()

    def dscratch(self, name, shape, dt=F32):
        return self.nc.dram_tensor(name, list(shape), dt, kind="Internal").ap()

    def arena_init(self):
        rem = self.nc.sbuf_bytes_remaining
        nb = (rem - 1024) // 64 * 64
        self.arena_bytes = nb
        self.arena = self.nc.alloc_sbuf_tensor("arena", [128, nb // 4], F32)

    def carve(self, off, shape, dt):
        esz = {F32: 4, I32: 4, U32: 4, BF16: 2}[dt]
        n = 1
        for v in shape[1:]:
            n *= v
        nb = (n * esz + 31) // 32 * 32
        assert off % 32 == 0
        assert off + nb <= self.arena_bytes, ("arena overflow", off + nb, self.arena_bytes)
        ap = self.arena[0:shape[0], off // 4:(off + nb) // 4]
        if dt != F32:
            ap = ap.bitcast(dt)
        ap = ap[:, 0:n]
        if len(shape) == 3:
            ap = ap.rearrange("p (a b) -> p a b", a=shape[1])
        elif len(shape) == 4:
            ap = ap.rearrange("p (a b c) -> p a b c", a=shape[1], b=shape[2])
        return ap, off + nb

    def mm(self, out, lhsT, rhs, start=True, stop=True, R=(), W=(), Wadd=()):
        return self.fw.op("pe", lambda e: e.matmul(out, lhsT, rhs, start=start, stop=stop), R=R, W=W, Wadd=Wadd)

    def tr(self, out, in_, ident, R=(), W=(), Wadd=()):
        return self.fw.op("pe", lambda e: e.transpose(out=out, in_=in_, identity=ident), R=R, W=W, Wadd=Wadd)

    def act(self, out, in_, func, R=(), W=(), Wadd=(), **kw):
        return self.fw.op("act", lambda e: e.activation(out=out, in_=in_, func=func, **kw), R=R, W=W, Wadd=Wadd)

    def ts(self, eng, out, in0, s1, s2, op0, op1=None, R=(), W=(), Wadd=()):
        if op1 is None:
            return self.fw.op(eng, lambda e: e.tensor_scalar(out=out, in0=in0, scalar1=s1, scalar2=None, op0=op0), R=R, W=W, Wadd=Wadd)
        return self.fw.op(eng, lambda e: e.tensor_scalar(out=out, in0=in0, scalar1=s1, scalar2=s2, op0=op0, op1=op1), R=R, W=W, Wadd=Wadd)

    def tt(self, eng, out, in0, in1, op, R=(), W=(), Wadd=()):
        return self.fw.op(eng, lambda e: e.tensor_tensor(out=out, in0=in0, in1=in1, op=op), R=R, W=W, Wadd=Wadd)

    def stt(self, out, in0, scalar, in1, op0, op1, R=(), W=(), Wadd=()):
        return self.fw.op("dve", lambda e: e.scalar_tensor_tensor(out=out, in0=in0, scalar=scalar, in1=in1, op0=op0, op1=op1), R=R, W=W, Wadd=Wadd)

    def cp(self, eng, out, in_, R=(), W=(), Wadd=()):
        if eng == "act":
            return self.act(out, in_, AF.Copy, R=R, W=W, Wadd=Wadd)
        return self.fw.op(eng, lambda e: e.tensor_copy(out=out, in_=in_), R=R, W=W, Wadd=Wadd)

    def barrier(self):
        fw = self.fw
        for e in ("pe", "act", "dve", "pool", "sp"):
            for q in fw.rings:
                for slot in fw.rings[q]["slots"]:
                    if slot[1] > 0:
                        fw._wait(e, slot[0], 16 * slot[1])
            for e2 in fw.sem:
                if fw.cnt[e2] > 0 and e2 != e:
                    fw._wait(e, fw.sem[e2], fw.cnt[e2])

    def rstd_from_sum(self, out, in_, n, R=(), W=()):
        self.act(out, in_, AF.Ln, scale=1.0 / n, bias=self.eps_col[0:out.shape[0], 0:1], R=list(R) + [self.t_const], W=W)
        self.act(out, out, AF.Exp, scale=-0.5, R=W, W=W)

    def build(self):
        nc, fw, S, L = self.nc, self.fw, self.S, self.L
        NB, NT = self.NB, self.NT
        x_in = self.din("x", [S, D])
        c_in = self.din("c", [8, 128])
        pos_in = self.din("pos", [1, S], I32)
        cst_in = self.din("cst", [128, 8])
        w_ada = self.din("w_ada", [L, D, 6 * D])
        b_ada = self.din("b_ada", [L, 6 * D])
        vecs = self.din("vecs", [L, 96, 128])
        lbl = self.din("lbl", [2 * L, 128])
        w_in = self.din("w_in", [L, D, IN_COLS])
        w_uq = self.din("w_uq", [L, 192, 384])
        w_ukv = self.din("w_ukv", [L, 128, 512])
        w_pool = self.din("w_pool", [L, 4, 64, 64])
        w_out = self.din("w_out", [L, D, D])
        rw = self.din("rw", [L, D, 36])
        rb = self.din("rb", [L, 36])
        rows = self.din("rows", [L, 2, D])
        w1r = self.din("w1r", [L * NEXP * 128, 8 * DEXP])
        w3r = self.din("w3r", [L * NEXP * 128, 8 * DEXP])
        w2r = self.din("w2r", [L * NEXP * 128, 4 * D])
        out = nc.dram_tensor("out", [S, D], F32, kind="ExternalOutput").ap()
        self.out = out
        dbg_y = None
        if self.dbg:
            dbg_y = nc.dram_tensor("dbg_y", [D, S], F32, kind="ExternalOutput").ap()
        modrow = self.dscratch("modrow", [L, 6 * D])
        ropeC = self.dscratch("ropeC", [32, S])
        ropeS = self.dscratch("ropeS", [32, S])
        KTd = self.dscratch("KTd", [NB, 96, 4, BLK], BF16)
        Vxd = self.dscratch("Vxd", [NB, 128, BLK // 128, 384], BF16)
        h2d = self.dscratch("h2d", [S, D], BF16)
        slotd = self.dscratch("slotd", [self.NSLOT, 16], I32)
        ysd = self.dscratch("ysd", [self.NSLOT, D], F32)
        t_modrow, t_rope = Tok(), Tok()
        t_KT = [Tok() for _ in range(NB)]
        t_xd = [Tok() for _ in range(NT)]
        t_h2d, t_slotd, t_ysd = Tok(), Tok(), Tok()

        self.t_const = tcs = Tok("const")
        ii = self.sb([128, 128], I32, "ii")
        fw.op("pool", lambda e: e.iota(ii[:], pattern=[[1, 128]], base=0, channel_multiplier=-1), W=[tcs])
        dfl = self.sb([128, 128], F32, "dfl")
        self.cp("dve", dfl[:], ii[:], R=[tcs], W=[tcs])
        idf = self.sb([128, 128], F32, "idf")
        self.ts("dve", idf[:], dfl[:], 0.0, None, ALU.is_equal, R=[tcs], W=[tcs])
        idb = self.sb([128, 128], BF16, "idb")
        self.cp("dve", idb[:], idf[:], R=[tcs], W=[tcs])
        ones_b = self.sb([128, 128], BF16, "ones_b")
        fw.op("pool", lambda e: e.memset(ones_b[:], 1.0), W=[tcs])
        ones_f = self.sb([128, 128], F32, "ones_f")
        fw.op("pool", lambda e: e.memset(ones_f[:], 1.0), W=[tcs])
        self.eps_col = self.sb([128, 1], F32, "eps")
        fw.op("pool", lambda e: e.memset(self.eps_col[:], EPS), W=[tcs])
        ustrict = self.sb([128, 128], BF16, "ustrict")
        self.ts("dve", ustrict[:], dfl[:], 0.0, None, ALU.is_gt, R=[tcs], W=[tcs])
        bd64 = self.sb([128, 128], BF16, "bd64")
        fw.op("pool", lambda e: e.memset(bd64[:], 0.0), W=[tcs])
        fw.op("pool", lambda e: e.memset(bd64[0:64, 0:64], 1.0), W=[tcs])
        fw.op("pool", lambda e: e.memset(bd64[64:128, 64:128], 1.0), W=[tcs])
        hm_i = self.sb([128, 64], I32, "hm_i")
        for qd in range(2):
            fw.op("pool", lambda e, qd=qd: e.iota(hm_i[64 * qd:64 * qd + 64, :], pattern=[[1, 64]], base=0, channel_multiplier=-1), W=[tcs])
        hm_f = self.sb([128, 64], F32, "hm_f")
        self.cp("dve", hm_f[:], hm_i[:], R=[tcs], W=[tcs])
        hmask = self.sb([128, 64], F32, "hmask")
        self.ts("dve", hmask[:], hm_f[:], 0.0, None, ALU.is_ge, R=[tcs], W=[tcs])
        rmask = self.sb([128, BLK], F32, "rmask")
        fw.op("pool", lambda e: e.memset(rmask[:], 1.0), W=[tcs])
        fw.op("pool", lambda e: e.memset(rmask[:].rearrange("p (c t) -> p c t", t=64)[:, :, 0:1], 0.0), W=[tcs])
        mlo = self.sb([128, BLK], BF16, "mlo")
        fw.op("pool", lambda e: e.memset(mlo[:], 1.0), W=[tcs])
        fw.op("pool", lambda e: e.memset(mlo[:].rearrange("p (c t) -> p c t", t=64)[:, :, 32:64], 0.0), W=[tcs])
        am_i = self.sb([128, BLK], I32, "am_i")
        am_f = self.sb([128, BLK], F32, "am_f")
        amask = []
        for m in range(4):
            fw.op("pool", lambda e, m=m: e.iota(am_i[:], pattern=[[1, BLK]], base=-128 * m, channel_multiplier=-1), R=[tcs], W=[tcs])
            self.cp("dve", am_f[:], am_i[:], R=[tcs], W=[tcs])
            a = self.sb([128, BLK], BF16, "amask%d" % m)
            self.ts("dve", a[:], am_f[:], 0.0, None, ALU.is_ge, R=[tcs], W=[tcs])
            amask.append(a)
        tcol = self.sb([128, 128], F32, "tcol")
        tci = self.sb([128, 128], I32, "tci")
        fw.op("pool", lambda e: e.iota(tci[:], pattern=[[1, 128]], base=0, channel_multiplier=0), W=[tcs])
        self.cp("dve", tcol[:], tci[:], R=[tcs], W=[tcs])
        tmpa = self.sb([128, 128], F32, "tmpa")
        tmpb = self.sb([128, 128], F32, "tmpb")
        Bmain, B0, Bprev = [], [], []
        for w in (2, 4, 8, 16):
            bm = self.sb([128, 128], BF16, "bm%d" % w)
            b0 = self.sb([128, 128], BF16, "b0%d" % w)
            bp = self.sb([128, 128], BF16, "bp%d" % w)
            self.ts("dve", tmpa[:], dfl[:], 0.0, None, ALU.is_ge, R=[tcs], W=[tcs])
            self.stt(tmpa[:], dfl[:], float(w - 1), tmpa[:], ALU.is_le, ALU.mult, R=[tcs], W=[tcs])
            self.stt(bm[:], tmpa[:], 1.0 / w, idf[:], ALU.mult, ALU.subtract, R=[tcs], W=[tcs])
            self.ts("dve", tmpb[:], tcol[:], 1.0, float(w), ALU.add, ALU.min, R=[tcs], W=[tcs])
            fw.op("dve", lambda e: e.reciprocal(out=tmpb[:], in_=tmpb[:]), R=[tcs], W=[tcs])
            self.tt("dve", tmpb[:], tmpa[:], tmpb[:], ALU.mult, R=[tcs], W=[tcs])
            self.tt("dve", b0[:], tmpb[:], idf[:], ALU.subtract, R=[tcs], W=[tcs])
            self.ts("dve", tmpa[:], dfl[:], 128.0, 1.0, ALU.add, ALU.is_ge, R=[tcs], W=[tcs])
            self.ts("dve", tmpb[:], dfl[:], 128.0, float(w - 1), ALU.add, ALU.is_le, R=[tcs], W=[tcs])
            self.stt(bp[:], tmpa[:], 1.0 / w, tmpb[:], ALU.mult, ALU.mult, R=[tcs], W=[tcs])
            Bmain.append(bm); B0.append(b0); Bprev.append(bp)
        cst = self.sb([128, 8], F32, "cst")
        fw.dma("sp", cst[:], cst_in[:, :], W=[tcs])

        RC = min(S, 1024)
        TWO_PI = 2.0 * np.pi
        C1 = 6.28125
        C2 = float(np.float32(TWO_PI - C1).view(np.uint32) & np.uint32(0xFFFFF000))
        C2 = float(np.array([np.float32(TWO_PI - C1).view(np.uint32) & np.uint32(0xFFFFF000)], np.uint32).view(np.float32)[0])
        C3 = float(TWO_PI - C1 - C2)
        MAGIC = 12582912.0
        c_sb = self.sb([8, 128], F32, "c_sb")
        cT = self.sb([128, 8], F32, "cT")
        ba = [self.sb([1, 512], F32, "ba%d" % i) for i in range(2)]
        mrow = [self.sb([1, 512], F32, "mrow%d" % i) for i in range(2)]
        lb_raw = self.sb([2 * L, 128], F32, "lb_raw")
        lbe = self.sb([128, L, 2], F32, "lbe")
        lbs = self.sb([128, 2], F32, "lbs")
        lbT = self.sb([128, L, 2], F32, "lbT")
        oml = self.sb([128, L, 2], F32, "oml")
        self.arena_init()
        o_ = 0
        rp_i, o_ = self.carve(o_, [96, RC], I32)
        rp_a, o_ = self.carve(o_, [96, RC], F32)
        rp_k, o_ = self.carve(o_, [96, RC], F32)
        rp_s, o_ = self.carve(o_, [96, RC], F32)
        trp = Tok()
        RR = slice(64, 96)
        for c0 in range(0, S, RC):
            fw.dma("sp", rp_i[64:96, :], pos_in[0, c0:c0 + RC].partition_broadcast(32), W=[trp])
            self.cp("dve", rp_a[64:96, :], rp_i[64:96, :], R=[trp], W=[trp])
            self.ts("dve", rp_a[64:96, :], rp_a[64:96, :], cst[64:96, 0:1], None, ALU.mult, R=[trp, tcs], W=[trp])
            self.ts("dve", rp_k[64:96, :], rp_a[64:96, :], 1.0 / TWO_PI, MAGIC, ALU.mult, ALU.add, R=[trp], W=[trp])
            self.ts("dve", rp_k[64:96, :], rp_k[64:96, :], MAGIC, None, ALU.subtract, R=[trp], W=[trp])
            self.stt(rp_a[64:96, :], rp_k[64:96, :], -C1, rp_a[64:96, :], ALU.mult, ALU.add, R=[trp], W=[trp])
            self.stt(rp_a[64:96, :], rp_k[64:96, :], -C2, rp_a[64:96, :], ALU.mult, ALU.add, R=[trp], W=[trp])
            self.stt(rp_a[64:96, :], rp_k[64:96, :], -C3, rp_a[64:96, :], ALU.mult, ALU.add, R=[trp], W=[trp])
            self.ts("dve", rp_a[64:96, :], rp_a[64:96, :], 3.141592, -3.141592, ALU.min, ALU.max, R=[trp], W=[trp])
            self.act(rp_s[64:96, :], rp_a[64:96, :], AF.Sin, R=[trp], W=[trp])
            self.ts("dve", rp_s[64:96, :], rp_s[64:96, :], cst[64:96, 1:2], None, ALU.mult, R=[trp, tcs], W=[trp])
            fw.dma("sp", ropeS[:, c0:c0 + RC], rp_s[64:96, :], R=[trp], W=[t_rope])
            self.act(rp_k[64:96, :], rp_a[64:96, :], AF.Abs, R=[trp], W=[trp])
            self.ts("dve", rp_k[64:96, :], rp_k[64:96, :], -1.0, float(np.pi / 2), ALU.mult, ALU.add, R=[trp], W=[trp])
            self.act(rp_k[64:96, :], rp_k[64:96, :], AF.Sin, R=[trp], W=[trp])
            fw.dma("sp", ropeC[:, c0:c0 + RC], rp_k[64:96, :], R=[trp], W=[t_rope])

        tc_ = Tok()
        fw.dma("sp", c_sb[:], c_in[:, :], W=[tc_])
        self.act(c_sb[:], c_sb[:], AF.Silu, R=[tc_], W=[tc_])
        bi_, pb, tpb = fw.bank()
        self.tr(pb[:, 0:8], c_sb[:], idf[0:8, 0:8], R=[tc_, tcs], W=[tpb])
        tcT = Tok()
        self.cp("dve", cT[:], pb[:, 0:8], R=[tpb], W=[tcT])
        wa = []
        for i in range(2):
            a_, o_ = self.carve(o_, [128, 8, 512], F32)
            wa.append(a_)
        twa = [Tok(), Tok()]
        tmrow = [Tok(), Tok()]
        it = 0
        for l in range(L):
            for cb in range(12):
                j = it % 2
                it += 1
                fw.dma("sp", wa[j][:], w_ada[l].rearrange("(k p) n -> p k n", p=128)[:, :, cb * 512:(cb + 1) * 512], W=[twa[j]])
                fw.dma("sp", ba[j][:], b_ada[l:l + 1, cb * 512:(cb + 1) * 512], W=[twa[j]])
                bi_, pb, tpb = fw.bank()
                for k in range(8):
                    self.mm(pb[0:1, :], cT[:, k:k + 1], wa[j][:, k, :], start=(k == 0), stop=False, R=[tcT, twa[j]], W=[tpb])
                self.mm(pb[0:1, :], ones_f[0:1, 0:1], ba[j][:], start=False, stop=True, R=[tcs, twa[j]], W=[tpb])
                self.cp("dve", mrow[j][:], pb[0:1, :], R=[tpb], W=[tmrow[j]])
                fw.dma("sp", modrow[l:l + 1, cb * 512:(cb + 1) * 512], mrow[j][:], R=[tmrow[j]], W=[t_modrow])

        tlb = Tok()
        fw.dma("sp", lb_raw[:], lbl[:, :], W=[tlb])
        bi_, pb, tpb = fw.bank()
        self.tr(pb[:, 0:2 * L], lb_raw[:], idf[0:2 * L, 0:2 * L], R=[tlb, tcs], W=[tpb])
        self.act(lbe[:].rearrange("p l c -> p (l c)"), pb[:, 0:2 * L], AF.Exp, R=[tpb], W=[tlb])
        self.cp("dve", lbs[:], lbe[:, 0, :], R=[tlb], W=[tlb])
        for l in range(1, L):
            self.tt("dve", lbs[:], lbs[:], lbe[:, l, :], ALU.add, R=[tlb], W=[tlb])
        fw.op("dve", lambda e: e.reciprocal(out=lbs[:], in_=lbs[:]), R=[tlb], W=[tlb])
        fw.op("pool", lambda e: e.memset(lbT[:, 0, :], 0.0), W=[tlb])
        for l in range(1, L):
            self.tt("dve", lbe[:, l, :], lbe[:, l, :], lbs[:], ALU.mult, R=[tlb], W=[tlb])
            self.tt("dve", lbT[:, l, :], lbT[:, l - 1, :], lbe[:, l, :], ALU.add, R=[tlb], W=[tlb])
        self.ts("dve", oml[:].rearrange("p l c -> p (l c)"), lbT[:].rearrange("p l c -> p (l c)"), -1.0, 1.0, ALU.mult, ALU.add, R=[tlb], W=[tlb])
        self.lbT, self.oml, self.tlb = lbT, oml, tlb

        self.barrier()
        self.c = dict(idf=idf[:], idb=idb[:], ones_b=ones_b[:], ones_f=ones_f[:], ustrict=ustrict[:], bd64=bd64[:], hmask=hmask[:],
                      rmask=rmask[:], mlo=mlo[:], amask=[a[:] for a in amask], Bmain=[a[:] for a in Bmain], B0=[a[:] for a in B0],
                      Bprev=[a[:] for a in Bprev], cst=cst[:])
        self.d = dict(x_in=x_in, vecs=vecs, w_in=w_in, w_uq=w_uq, w_ukv=w_ukv, w_pool=w_pool, w_out=w_out, rw=rw, rb=rb,
                      rows=rows, w1r=w1r, w3r=w3r, w2r=w2r, modrow=modrow, ropeC=ropeC, ropeS=ropeS, KTd=KTd, Vxd=Vxd,
                      h2d=h2d, slotd=slotd, ysd=ysd, dbg_y=dbg_y)
        self.t = dict(modrow=t_modrow, rope=t_rope, KT=t_KT, xd=t_xd, h2d=t_h2d, slotd=t_slotd, ysd=t_ysd, dbg=Tok())
        self.alloc_layer_buffers()
        self.bc_reg = nc.gpsimd.to_reg(L * NEXP * 128 - 1)
        for l in range(L):
            self.layer(l)
        fw.finish(self.t["xd"] + [self.t["dbg"]])
        return nc

    def alloc_layer_buffers(self):
        B, T = {}, {}
        self.B, self.T = B, T
        NT, NTB, NBK = self.NT, BLK // 128, self.NBLKS
        off = {"P": 0}

        def mk(region, name, shape, dt, n=1):
            aps, toks = [], []
            for _ in range(n):
                ap, off[region] = self.carve(off[region], shape, dt)
                aps.append(ap)
                toks.append(Tok(name))
            B[name] = aps[0] if n == 1 else aps
            T[name] = toks[0] if n == 1 else toks
        P = "P"
        mk(P, "w_in", [128, 8, IN_COLS], BF16)
        mk(P, "w_out", [128, 8, D], BF16)
        mk(P, "w_uq", [128, 2, 384], BF16)
        mk(P, "w_uqs", [128, 2, 4, 96], BF16)
        mk(P, "wk", [128, 4, 96], BF16)
        mk(P, "wv", [128, 4, 64], BF16)
        mk(P, "wpl", [128, 2, 128], BF16)
        mk(P, "dgw", [128, 2, 31, 128], BF16)
        mk(P, "rwf", [128, 8, 36], F32)
        mk(P, "rbb", [128, 36], F32)
        mk(P, "vraw", [96, 128], F32)
        mk(P, "vT", [128, 96], F32)
        mk(P, "mraw", [48, 128], F32)
        mk(P, "mT", [128, 48], F32)
        mk(P, "sc1", [128, 8], F32)
        mk(P, "G2row", [128, D], F32)
        mk(P, "sh2row", [128, D], F32)
        mk(P, "g2row", [128, D], F32)
        mk(P, "hcol", [128, 8], F32)
        mk(P, "Bk", [128, 2, 96], BF16)
        mk(P, "A1", [128, NT, 32], BF16)
        mk(P, "A2", [128, NT, 32], BF16)
        mk(P, "r12", [128, NT, 2], F32)
        mk(P, "pw", [128, NT, 2], F32)
        mk(P, "carry", [128, 32], F32)
        mk(P, "tokid", [128, NT], I32)
        mk(P, "pidx", [128, 1], F32)
        mk(P, "bst", [128, NBK], F32)
        mk(P, "zslot", [128, NBK], I32)
        off["A"] = off["M"] = off["P"]
        A = "A"
        mk(A, "xt", [128, D], F32, 2)
        mk(A, "xn", [128, D], BF16, 2)
        mk(A, "st", [128, 16], F32)
        mk(A, "junk", [128, D], BF16)
        mk(A, "hT", [128, 8, BLK], BF16)
        mk(A, "QT", [96, 4, BLK], BF16)
        mk(A, "KTb", [96, 4, BLK], BF16)
        mk(A, "Vxb", [128, NTB, 384], BF16)
        mk(A, "KTl", [96, 4, BLK], BF16, 2)
        mk(A, "Vxl", [128, NTB, 384], BF16, 2)
        mk(A, "PT", [128, BLK], BF16, 3)
        mk(A, "yT", [128, 8, BLK], BF16)
        mk(A, "hQt", [128, 2, BLK], BF16)
        mk(A, "hKt", [128, 2, BLK], BF16)
        mk(A, "hKtT", [128, NTB, 256], BF16)
        mk(A, "hKlo", [128, 2, BLK], BF16)
        mk(A, "hv", [128, NTB, 256], BF16)
        mk(A, "hgb", [128, 2, BLK], BF16)
        mk(A, "hgc", [128, 2, 2, BLK // 64], F32)
        mk(A, "hAm", [128, NTB, 4, 128], BF16)
        mk(A, "hS", [128, 2, 64], F32)
        mk(A, "hSb", [128, 2, BLK // 64, 128], BF16)
        mk(A, "cqn1", [128, BLK], BF16)
        mk(A, "krb", [128, BLK], BF16)
        mk(A, "hSp", [128, 2, 64], F32)
        mk(A, "cu", [128, 2, 32 + BLK], BF16)
        mk(A, "pu", [128, NTB + 1, 256], BF16)
        mk(A, "h2b", [128, D], BF16, 2)
        mk(A, "h2T", [128, 8, 128], F32)
        mk(A, "lg", [128, 36], F32)
        mk(A, "rt", [128, 96], F32)
        mk(A, "Ab", [128, 32], BF16, 2)
        self.NF, self.NH, self.ND = 10, 9, 3
        mk(A, "F", [128, BLK], F32, self.NF)
        mk(A, "H", [128, BLK], BF16, self.NH)
        mk(A, "Dd", [128, D], F32, self.ND)
        self.pool_free = {"F": list(range(self.NF)), "H": list(range(self.NH)), "Dd": list(range(self.ND))}
        self.tmp_stack = []
        M = "M"
        mk(M, "cnt_i", [128, 32], I32)
        mk(M, "pad_f", [128, 32], F32)
        mk(M, "pend", [128, 32], F32)
        mk(M, "base", [128, 32], F32)
        mk(M, "bexp", [128, NBK], F32)
        mk(M, "bchg", [128, NBK], F32)
        mk(M, "btmp", [128, NBK], F32)
        mk(M, "widx", [128, NBK], I32)
        mk(M, "slot", [128, NT, 2], F32)
        mk(M, "sloti", [128, NT, 2], I32)
        mk(M, "t32", [128, 32], F32, 2)
        mk(M, "wst", [128, 4096], F32, 1)
        mk(M, "w1", [128, 8, DEXP], BF16)
        mk(M, "w3", [128, 8, DEXP], BF16)
        mk(M, "w2", [128, 4, D], BF16)
        mk(M, "tki", [128, 16], I32, 2)
        mk(M, "tk16", [128, 16], I32, 2)
        mk(M, "zz", [128, 1024], I32)
        mk(M, "xb", [128, D], BF16, 2)
        mk(M, "xbT", [128, 8, 128], BF16, 2)
        mk(M, "h1", [128, 4, 128], F32)
        mk(M, "hbT", [128, 4, 128], BF16, 2)
        mk(M, "yb", [128, D], F32, 2)
        mk(M, "y1", [128, D], F32, 2)
        mk(M, "y2", [128, D], F32, 1)
        mk(M, "xm", [128, D], F32, 1)
        for nm_ in ("y2", "xm"):
            B[nm_] = [B[nm_], B[nm_]]
            T[nm_] = [T[nm_], T[nm_]]
        self.region_end = dict(off)
        fw, c = self.fw, self.c
        fw.op("pool", lambda e: e.memset(B["Bk"], 0.0), W=[T["Bk"]])
        self.cp("dve", B["Bk"][0:32, 0, 64:96], c["idb"][0:32, 0:32], R=[self.t_const], W=[T["Bk"]])
        self.cp("dve", B["Bk"][0:32, 1, 64:80], c["idb"][0:32, 16:32], R=[self.t_const], W=[T["Bk"]])
        self.cp("dve", B["Bk"][0:32, 1, 80:96], c["idb"][0:32, 0:16], R=[self.t_const], W=[T["Bk"]])
        fw.op("pool", lambda e: e.memset(B["w_uqs"], 0.0), W=[T["w_uqs"]])
        fw.op("pool", lambda e: e.memset(B["wk"], 0.0), W=[T["wk"]])
        fw.op("pool", lambda e: e.iota(B["zslot"][:, 0:1], pattern=[[1, 1]], base=0, channel_multiplier=1), W=[T["zslot"]])
        self.cp("dve", B["pidx"], B["zslot"][:, 0:1], R=[T["zslot"]], W=[T["pidx"]])
        fw.op("pool", lambda e: e.iota(B["tokid"], pattern=[[128, NT]], base=0, channel_multiplier=1), W=[T["tokid"]])
        fw.op("pool", lambda e: e.iota(B["zslot"], pattern=[[128, NBK]], base=0, channel_multiplier=0), R=[T["pidx"]], W=[T["zslot"]])
        self.cp("dve", B["bst"], B["zslot"], R=[T["zslot"]], W=[T["bst"]])
        fw.op("pool", lambda e: e.memset(B["zslot"], 0), R=[T["bst"]], W=[T["zslot"]])

    def tmp(self, kind):
        free = self.pool_free[kind]
        assert free, "out of temp slots " + kind
        i = free.pop(0)
        self.tmp_stack.append((kind, i))
        return self.B[kind][i], self.T[kind][i]

    def tmp_mark(self):
        return len(self.tmp_stack)

    def tmp_release(self, mark):
        while len(self.tmp_stack) > mark:
            kind, i = self.tmp_stack.pop()
            self.pool_free[kind].append(i)

    def prologue(self, l):
        fw, B, T, c, d = self.fw, self.B, self.T, self.c, self.d
        tcs = self.t_const
        fw.dma("sp", B["vraw"], d["vecs"][l], W=[T["vraw"]])
        bi_, pb, tpb = fw.bank()
        self.tr(pb[:, 0:96], B["vraw"], c["idf"][0:96, 0:96], R=[T["vraw"], tcs], W=[tpb])
        self.cp("dve", B["vT"], pb[:, 0:96], R=[tpb], W=[T["vT"]])
        fw.dma("sp", B["mraw"], d["modrow"][l].rearrange("(j p) -> j p", p=128), R=[self.t["modrow"]], W=[T["mraw"]])
        bi_, pb, tpb = fw.bank()
        self.tr(pb[:, 0:48], B["mraw"], c["idf"][0:48, 0:48], R=[T["mraw"], tcs], W=[tpb])
        self.cp("dve", B["mT"], pb[:, 0:48], R=[tpb], W=[T["mT"]])
        self.stt(B["sc1"], B["mT"][:, 8:16], 1.0, B["vT"][:, 0:8], ALU.add, ALU.mult, R=[T["mT"], T["vT"]], W=[T["sc1"]])
        mr = d["modrow"]
        mark = self.tmp_mark()
        g1row, tg1 = self.tmp("Dd")
        n2row, tn2 = self.tmp("Dd")
        fw.dma("sp", g1row, mr[l, 2 * D:3 * D].partition_broadcast(128), R=[self.t["modrow"]], W=[tg1])
        fw.dma("sp", B["g2row"], mr[l, 5 * D:6 * D].partition_broadcast(128), R=[self.t["modrow"]], W=[T["g2row"]])
        fw.dma("sp", B["sh2row"], mr[l, 3 * D:4 * D].partition_broadcast(128), R=[self.t["modrow"]], W=[T["sh2row"]])
        fw.dma("sp", B["G2row"], mr[l, 4 * D:5 * D].partition_broadcast(128), R=[self.t["modrow"]], W=[T["G2row"]])
        fw.dma("sp", n2row, d["rows"][l, 0, :].partition_broadcast(128), W=[tn2])
        self.stt(B["G2row"], B["G2row"], 1.0, n2row, ALU.add, ALU.mult, R=[T["G2row"], tn2], W=[T["G2row"]])
        wi = d["w_in"][l].rearrange("(k p) n -> p k n", p=128)
        fw.dma("pool", B["w_in"][:, :, 0:1072], wi[:, :, 0:1072], W=[T["w_in"]])
        fw.dma("pool", B["w_in"][:, :, 1072:IN_COLS], wi[:, :, 1072:IN_COLS], Wadd=[T["w_in"]])
        fw.dma("pool", B["w_out"], d["w_out"][l].rearrange("(k p) n -> p k n", p=128), W=[T["w_out"]])
        for k in range(8):
            self.tt("dve" if k % 2 else "pool", B["w_out"][:, k, :], B["w_out"][:, k, :], g1row, ALU.mult,
                    R=[tg1], W=[T["w_out"]])
        self.tmp_release(mark)
        wq = d["w_uq"][l]
        fw.op("pool", lambda e: e.memset(B["w_uq"][:, 1, :], 0.0), W=[T["w_uq"]])
        fw.dma("pool", B["w_uq"][:, 0, :], wq[0:128, :], Wadd=[T["w_uq"]])
        fw.dma("pool", B["w_uq"][0:64, 1, :], wq[128:192, :], Wadd=[T["w_uq"]])
        wq3 = wq.rearrange("r (h c) -> r h c", c=96)
        fw.sync("pool", W=[T["w_uqs"]])
        fw.dma("pool", B["w_uqs"][:, 0, :, 64:80], wq3[0:128, :, 80:96], Wadd=[T["w_uqs"]])
        fw.dma("pool", B["w_uqs"][:, 0, :, 80:96], wq3[0:128, :, 64:80], Wadd=[T["w_uqs"]])
        fw.dma("pool", B["w_uqs"][0:64, 1, :, 64:80], wq3[128:192, :, 80:96], Wadd=[T["w_uqs"]])
        fw.dma("pool", B["w_uqs"][0:64, 1, :, 80:96], wq3[128:192, :, 64:80], Wadd=[T["w_uqs"]])
        wkv3 = d["w_ukv"][l].rearrange("r (h c) -> r h c", c=128)
        fw.sync("pool", W=[T["wk"]])
        fw.dma("pool", B["wk"][:, :, 0:64], wkv3[:, :, 0:64], Wadd=[T["wk"]])
        fw.dma("pool", B["wv"], wkv3[:, :, 64:128], W=[T["wv"]])
        fw.op("pool", lambda e: e.memset(B["wpl"], 0.0), W=[T["wpl"]])
        for g in range(4):
            p0 = 64 * (g % 2)
            fw.dma("pool", B["wpl"][p0:p0 + 64, g // 2, p0:p0 + 64], d["w_pool"][l, g], Wadd=[T["wpl"]])
        fw.dma("sp", B["rwf"], d["rw"][l].rearrange("(k p) n -> p k n", p=128), W=[T["rwf"]])
        fw.dma("sp", B["rbb"], d["rb"][l, :].partition_broadcast(128), W=[T["rbb"]])
        for ct in range(2):
            for j in range(31):
                col = 32 + 2 * j + ct
                self.ts("pool" if (j % 2) else "dve", B["dgw"][:, ct, j, :], c["idb"], B["vT"][:, col:col + 1], None, ALU.mult,
                        R=[T["vT"], tcs], W=[T["dgw"]])
        hc = B["hcol"]
        self.cp("dve", hc[:, 0:2], self.lbT[:, l, :], R=[self.tlb], W=[T["hcol"]])
        self.cp("dve", hc[:, 2:4], self.oml[:, l, :], R=[self.tlb], W=[T["hcol"]])
        self.ts("dve", hc[:, 4:6], self.oml[:, l, :], -1.0, None, ALU.mult, R=[self.tlb], W=[T["hcol"]])
        fw.op("pool", lambda e: e.memset(B["hS"], 0.0), W=[T["hS"]])
        fw.op("pool", lambda e: e.memset(B["hSb"], 0.0), W=[T["hSb"]])
        fw.op("pool", lambda e: e.memset(B["cu"], 0.0), W=[T["cu"]])
        fw.op("pool", lambda e: e.memset(B["pu"], 0.0), W=[T["pu"]])
        fw.op("pool", lambda e: e.memset(B["carry"], 0.0), W=[T["carry"]])
        fw.op("pool", lambda e: e.memset(B["Vxb"], 1.0), W=[T["Vxb"]])
        fw.op("pool", lambda e: e.memset(B["cqn1"], 0.0), W=[T["cqn1"]])
        fw.op("pool", lambda e: e.memset(B["krb"], 0.0), W=[T["krb"]])
        fw.op("pool", lambda e: e.memset(B["hAm"], 0.0), W=[T["hAm"]])

    def layer(self, l):
        if STAGE < 1:
            return
        self.prologue(l)
        if STAGE < 2:
            return
        for bi in range(self.NB):
            self.mixer_block(l, bi)
        self.barrier()
        if STAGE < 9:
            return
        self.moe(l)
        self.barrier()

    def proj(self, col0, width):
        B, T = self.B, self.T
        bi_, pb, tpb = self.fw.bank(hold=True)
        for k in range(8):
            self.mm(pb[0:width, 0:BLK], B["w_in"][:, k, col0:col0 + width], B["hT"][:, k, :], start=(k == 0), stop=(k == 7),
                    R=[T["w_in"], T["hT"]], W=[tpb])
        return bi_, pb, tpb

    def sumsq_rstd(self, parts, n, width):
        c = self.c
        rs, trs = self.tmp("F")
        m_ = self.tmp_mark()
        sqs = []
        kmax = max(w for _, w, _ in parts)
        ksq = os.environ.get("KSQ", "")
        for ap, w, tk in parts:
            sq, tsq = self.tmp("H")
            if w < kmax and "m" not in ksq:
                self.fw.op("pool", lambda e, sq=sq, w=w: e.memset(sq[w:kmax, :], 0.0), W=[tsq])
            if ("a" in ksq and w == 128) or ("b" in ksq and w == 64):
                pass
            elif "d" in ksq:
                self.cp("dve", sq[0:w, :], ap, R=[tk], W=[tsq])
            elif "c" in ksq:
                self.act(sq[0:w, :], ap, AF.Copy, R=[tk], W=[tsq])
            elif "j" in ksq:
                self.act(self.B["junk"][0:w, 0:BLK], ap, AF.Square, R=[tk], W=[self.T["junk"]])
            elif "h" in ksq:
                self.act(sq[0:w, :], self.B["hT"][0:w, 0, :], AF.Square, R=[self.T["hT"]], W=[tsq])
            elif "s" in ksq:
                self.fw.sync("act", R=[tk], W=[tsq])
                self.fw.op("act", lambda e, sq=sq, w=w, ap=ap: e.activation(out=sq[0:w, :], in_=ap, func=AF.Square))
            else:
                self.act(sq[0:w, :], ap, AF.Square, R=[tk], W=[tsq])
            sqs.append((sq, w, tsq))
        bi_, ps, tps = self.fw.bank()
        if os.environ.get("KNOMM"):
            self.tmp_release(m_)
            return rs, trs
        for i, (sq, w, tsq) in enumerate(sqs):
            self.mm(ps[0:width, 0:BLK], c["ones_b"][0:kmax, 0:width], sq[0:kmax, :], start=(i == 0), stop=(i == len(sqs) - 1),
                    R=[tsq, self.t_const], W=[tps])
        self.tmp_release(m_)
        if os.environ.get("KNORS"):
            self.cp("dve", rs[0:width, :], ps[0:width, 0:BLK], R=[tps], W=[trs])
            return rs, trs
        self.rstd_from_sum(rs[0:width, :], ps[0:width, 0:BLK], n, R=[tps], W=[trs])
        return rs, trs

    def mixer_block(self, l, bi):
        fw, B, T, c, d = self.fw, self.B, self.T, self.c, self.d
        tcs = self.t_const
        NTB = BLK // 128
        t0 = bi * BLK
        xsrc = d["x_in"] if l == 0 else self.out
        vT, tvT = B["vT"], T["vT"]
        st, tst = B["st"], T["st"]
        for t in range(NTB):
            j = t % 2
            tile = bi * NTB + t
            xt, txt = B["xt"][j], T["xt"][j]
            fw.dma("sp", xt, xsrc[t0 + t * 128:t0 + (t + 1) * 128, :], R=[self.t["xd"][tile]], W=[txt])
            self.act(B["junk"], xt, AF.Square, R=[txt], W=[T["junk"]])
            fw.op("dve", lambda e, t=t: e.tensor_reduce(out=st[:, t:t + 1], in_=B["junk"], axis=mybir.AxisListType.X, op=ALU.add),
                  R=[T["junk"]], W=[tst])
            self.rstd_from_sum(st[:, 4 + t:5 + t], st[:, t:t + 1], D, R=[tst], W=[tst])
            xn, txn = B["xn"][j], T["xn"][j]
            self.ts("dve", xn, xt, st[:, 4 + t:5 + t], None, ALU.mult, R=[txt, tst], W=[txn])
            bi_, pb, tpb = fw.bank()
            pbb = pb[:].bitcast(BF16)
            for k in range(8):
                self.tr(pbb[:, k * 128:(k + 1) * 128], xn[:, k * 128:(k + 1) * 128], c["idb"], R=[txn, tcs], W=[tpb])
            for k in range(8):
                o_ = B["hT"][:, k, t * 128:(t + 1) * 128]
                i_ = pbb[:, k * 128:(k + 1) * 128]
                if k % 2 == 0:
                    self.act(o_, i_, AF.Identity, scale=B["sc1"][:, k:k + 1], bias=B["mT"][:, k:k + 1],
                             R=[tpb, T["sc1"], T["mT"]], W=[T["hT"]])
                else:
                    self.ts("dve", o_, i_, B["sc1"][:, k:k + 1], B["mT"][:, k:k + 1], ALU.mult, ALU.add,
                            R=[tpb, T["sc1"], T["mT"]], W=[T["hT"]])
        if STAGE < 2.5:
            return
        if os.environ.get("KBAR"):
            self.barrier()
        mark = self.tmp_mark()
        cq0, tcq0 = self.tmp("H")
        cq1, tcq1 = self.tmp("H")
        ba_, pa, tpa = self.proj(0, 128)
        bb_, pbk, tpbk = self.proj(128, 64)
        self.cp("dve", cq0, pa[:, 0:BLK], R=[tpa], W=[tcq0])
        self.cp("dve", cq1[0:64, :], pbk[0:64, 0:BLK], R=[tpbk], W=[tcq1])
        if os.environ.get("KBAR2"):
            self.barrier()
        if STAGE < 2.6:
            fw.release(ba_); fw.release(bb_); self.tmp_release(mark); return
        rs, trs = self.sumsq_rstd([(pa[:, 0:BLK], 128, tpa), (pbk[0:64, 0:BLK], 64, tpbk)], 192, 128)
        fw.release(ba_); fw.release(bb_)
        if STAGE < 2.7:
            self.tmp_release(mark); return
        cqn0, tcqn0 = self.tmp("H")
        cqn1, tcqn1 = B["cqn1"], T["cqn1"]
        self.stt(cqn0, cq0, vT[:, 16:17], rs, ALU.mult, ALU.mult, R=[tcq0, tvT, trs], W=[tcqn0])
        self.stt(cqn1[0:64, :], cq1[0:64, :], vT[0:64, 17:18], rs[0:64, :], ALU.mult, ALU.mult, R=[tcq1, tvT, trs], Wadd=[tcqn1])
        if STAGE < 3.1:
            self.tmp_release(mark); return
        bc_, pc, tpc = self.proj(192, 128)
        ckr, tckr = self.tmp("H")
        self.cp("dve", ckr, pc[:, 0:BLK], R=[tpc], W=[tckr])
        rs2, trs2 = self.sumsq_rstd([(pc[:, 0:BLK], 128, tpc)], 128, 128)
        fw.release(bc_)
        ckvn, tckvn = self.tmp("H")
        self.stt(ckvn, ckr, vT[:, 18:19], rs2, ALU.mult, ALU.mult, R=[tckr, tvT, trs2], W=[tckvn])
        bk_, pk, tpk = self.proj(320, 32)
        krb, tkrb = B["krb"], T["krb"]
        self.cp("dve", krb[0:32, :], pk[0:32, 0:BLK], R=[tpk], Wadd=[tkrb])
        fw.release(bk_)
        rC, trC = self.tmp("F")
        rS, trS = self.tmp("F")
        fw.dma("sp", rC[64:96, :], d["ropeC"][:, t0:t0 + BLK], R=[self.t["rope"]], W=[trC])
        fw.dma("sp", rS[64:96, :], d["ropeS"][:, t0:t0 + BLK], R=[self.t["rope"]], W=[trS])
        if STAGE < 3.2:
            self.tmp_release(mark); return
        bi_, pks, tpks = fw.bank()
        self.mm(pks[0:96, 0:BLK], B["Bk"][:, 1, :], krb, R=[T["Bk"], tkrb], W=[tpks])
        ksw, tksw = self.tmp("F")
        self.cp("dve", ksw[64:96, :], pks[64:96, 0:BLK], R=[tpks], W=[tksw])

        def head_norm_rope(pq, tpq, src_sw, tsw, gcol, gscol, dst, tdst, h, sw_is_psum):
            m2 = self.tmp_mark()
            rq, trq = self.sumsq_rstd([(pq[0:96, 0:BLK], 96, tpq)], 96, 96)
            qn, tqn = self.tmp("F")
            qs, tqs = self.tmp("F")
            self.stt(qn[0:96, :], pq[0:96, 0:BLK], vT[0:96, gcol:gcol + 1], rq[0:96, :], ALU.mult, ALU.mult, R=[tpq, tvT, trq], W=[tqn])
            self.stt(qs[64:96, :], src_sw, vT[64:96, gscol:gscol + 1], rq[64:96, :], ALU.mult, ALU.mult, R=[tsw, tvT, trq], W=[tqs])
            self.cp("act", dst[0:64, h, :], qn[0:64, :], R=[tqn], Wadd=[tdst])
            self.tt("pool", qs[64:96, :], qs[64:96, :], rS[64:96, :], ALU.mult, R=[trS], W=[tqs])
            self.tt("pool", qn[64:96, :], qn[64:96, :], rC[64:96, :], ALU.mult, R=[trC], W=[tqn])
            self.tt("dve", dst[64:96, h, :], qn[64:96, :], qs[64:96, :], ALU.add, R=[tqn, tqs], Wadd=[tdst])
            self.tmp_release(m2)

        if STAGE < 3.3:
            self.tmp_release(mark); return
        fw.sync("act", W=[T["QT"]]); fw.sync("dve", W=[T["QT"]])
        fw.sync("act", W=[T["KTb"]]); fw.sync("dve", W=[T["KTb"]])
        for h in range(4):
            bq_, pq, tpq = fw.bank(hold=True)
            self.mm(pq[0:96, 0:BLK], B["w_uq"][:, 0, h * 96:(h + 1) * 96], cqn0, start=True, stop=False, R=[T["w_uq"], tcqn0], W=[tpq])
            self.mm(pq[0:96, 0:BLK], B["w_uq"][:, 1, h * 96:(h + 1) * 96], cqn1, start=False, stop=True, R=[T["w_uq"], tcqn1], W=[tpq])
            bs_, pqs, tpqs = fw.bank(hold=True)
            self.mm(pqs[0:96, 0:BLK], B["w_uqs"][:, 0, h, :], cqn0, start=True, stop=False, R=[T["w_uqs"], tcqn0], W=[tpqs])
            self.mm(pqs[0:96, 0:BLK], B["w_uqs"][:, 1, h, :], cqn1, start=False, stop=True, R=[T["w_uqs"], tcqn1], W=[tpqs])
            head_norm_rope(pq, tpq, pqs[64:96, 0:BLK], tpqs, 19, 20, B["QT"], T["QT"], h, True)
            fw.release(bq_); fw.release(bs_)
        if STAGE < 3.4:
            self.tmp_release(mark); return
        for h in range(4):
            bq_, pq, tpq = fw.bank(hold=True)
            self.mm(pq[0:96, 0:BLK], B["wk"][:, h, :], ckvn, start=True, stop=False, R=[T["wk"], tckvn], W=[tpq])
            self.mm(pq[0:96, 0:BLK], B["Bk"][:, 0, :], krb, start=False, stop=True, R=[T["Bk"], tkrb], W=[tpq])
            head_norm_rope(pq, tpq, ksw[64:96, :], tksw, 21, 22, B["KTb"], T["KTb"], h, False)
            fw.release(bq_)
        fw.dma("sp", d["KTd"][bi], B["KTb"], R=[T["KTb"]], W=[self.t["KT"][bi]])
        if STAGE < 3.5:
            self.tmp_release(mark); return
        vcol = [0, 128, 192, 320]
        fw.sync("dve", W=[T["Vxb"]]); fw.sync("act", W=[T["Vxb"]])
        for t in range(NTB):
            bi_, pv, tpv = fw.bank()
            self.mm(pv[:, 0:256], ckvn[:, t * 128:(t + 1) * 128], B["wv"].rearrange("p h c -> p (h c)"), R=[tckvn, T["wv"]], W=[tpv])
            for h in range(4):
                self.cp("dve" if h % 2 else "act", B["Vxb"][:, t, vcol[h]:vcol[h] + 64], pv[:, h * 64:(h + 1) * 64], R=[tpv], Wadd=[T["Vxb"]])
        fw.dma("sp", d["Vxd"][bi], B["Vxb"], R=[T["Vxb"]], Wadd=[self.t["KT"][bi]])
        self.tmp_release(mark)
        if STAGE < 4:
            return
        scale = 96.0 ** -0.5
        vsl = [(0, 128), (64, 192), (192, 320), (256, 384)]
        obs = [fw.bank(hold=True) for _ in range(4)]
        pti = 0
        for jb in range(bi + 1):
            j = jb % 2
            fw.dma("sp", B["KTl"][j], d["KTd"][jb], R=[self.t["KT"][jb]], W=[T["KTl"][j]])
            fw.dma("sp", B["Vxl"][j], d["Vxd"][jb], R=[self.t["KT"][jb]], W=[T["Vxl"][j]])
            for h in range(4):
                ob_, ob, tob = obs[h]
                for kt in range(NTB):
                    bi_, ps, tps = fw.bank()
                    self.mm(ps[:, 0:BLK], B["KTl"][j][0:96, h, kt * 128:(kt + 1) * 128], B["QT"][0:96, h, :],
                            R=[T["KTl"][j], T["QT"]], W=[tps])
                    PT, tPT = B["PT"][pti % 3], T["PT"][pti % 3]
                    pti += 1
                    self.act(PT, ps[:, 0:BLK], AF.Exp, scale=scale, R=[tps], W=[tPT])
                    if jb == bi:
                        self.tt("pool", PT, PT, c["amask"][kt][:, 0:BLK], ALU.mult, R=[tcs], W=[tPT])
                    self.mm(ob[:, 0:BLK], B["Vxl"][j][:, kt, vsl[h][0]:vsl[h][1]], PT,
                            start=(jb == 0 and kt == 0), stop=(jb == bi and kt == NTB - 1), R=[T["Vxl"][j], tPT], W=[tob])
        mark = self.tmp_mark()
        fw.sync("dve", W=[T["yT"]]); fw.sync("act", W=[T["yT"]])
        for h in range(4):
            ob_, ob, tob = obs[h]
            nlo, dlo = ((0, 64), (64, 128)) if h % 2 == 0 else ((64, 128), (0, 64))
            rd, trd = self.tmp("F")
            rd2, trd2 = self.tmp("F")
            fw.op("dve", lambda e: e.reciprocal(out=rd[dlo[0]:dlo[1], :], in_=ob[dlo[0]:dlo[1], 0:BLK]), R=[tob], W=[trd])
            fw.dma("sp", rd2[nlo[0]:nlo[1], :], rd[dlo[0]:dlo[1], :], R=[trd], W=[trd2])
            self.tt("dve", B["yT"][nlo[0]:nlo[1], h // 2, :], ob[nlo[0]:nlo[1], 0:BLK], rd2[nlo[0]:nlo[1], :], ALU.mult,
                    R=[tob, trd2], Wadd=[T["yT"]])
            fw.release(ob_)
        self.tmp_release(mark)
        self.mixer_block2(l, bi)

    def mixer_block2(self, l, bi):
        skip = os.environ.get("KSKIP", "")
        if STAGE >= 5 and "hgrn" not in skip:
            self.mix_hgrn(l, bi)
        else:
            self.fw.op("pool", lambda e: e.memset(self.B["yT"][:, 4:6, :], 0.0), Wadd=[self.T["yT"]])
        if STAGE >= 6 and "conv" not in skip:
            self.mix_conv(l, bi)
        if STAGE >= 7 and "pool" not in skip:
            self.mix_pool(l, bi)
        self.mix_dbg(l, bi)
        if STAGE >= 8:
            self.mix_out(l, bi)

    def mix_hgrn(self, l, bi):
        fw, B, T, c, d = self.fw, self.B, self.T, self.c, self.d
        tcs = self.t_const
        NTB = BLK // 128
        NCH = BLK // 32
        t0 = bi * BLK
        xsrc = d["x_in"] if l == 0 else self.out
        vT, tvT = B["vT"], T["vT"]
        st, tst = B["st"], T["st"]
        hc, thc = B["hcol"], T["hcol"]
        NC6 = BLK // 64
        mark = self.tmp_mark()
        for ct in range(2):
            m2 = self.tmp_mark()
            bq_, phq, tphq = self.proj(608 + ct * 128, 128)
            q, tq = self.tmp("F")
            self.act(q, phq[:, 0:BLK], AF.Silu, R=[tphq], W=[tq])
            fw.release(bq_)
            bf_, phf, tphf = self.proj(864 + ct * 128, 128)
            sg, tsg = self.tmp("F")
            self.act(sg, phf[:, 0:BLK], AF.Sigmoid, R=[tphf], W=[tsg])
            fw.release(bf_)
            k, tk = self.tmp("F")
            self.ts("dve", k, sg, hc[:, 4 + ct:5 + ct], hc[:, 2 + ct:3 + ct], ALU.mult, ALU.add, R=[tsg, thc], W=[tk])
            self.act(sg, sg, AF.Ln, scale=hc[:, 2 + ct:3 + ct], bias=hc[:, ct:ct + 1], R=[thc], W=[tsg])
            bc, tbc = self.tmp("F")
            fw.op("dve", lambda e: e.tensor_tensor_scan(out=bc, data0=c["rmask"][:, 0:BLK], data1=sg, initial=0.0,
                                                        op0=ALU.mult, op1=ALU.add), R=[tsg, tcs], W=[tbc])
            b3 = bc.rearrange("p (c t) -> p c t", t=64)
            self.act(B["hgc"][:, ct, 0, :], b3[:, :, 31], AF.Exp, R=[tbc], Wadd=[T["hgc"]])
            bm, tbm = self.tmp("F")
            self.tt("dve", bm.rearrange("p (c t) -> p c t", t=64), b3, b3[:, :, 31:32].to_broadcast([128, NC6, 64]), ALU.subtract, R=[tbc], W=[tbm])
            self.ts("dve", bm, bm, 80.0, -80.0, ALU.min, ALU.max, R=[], W=[tbm])
            Ep, tEp = self.tmp("F")
            self.act(Ep, bm, AF.Exp, R=[tbm], W=[tEp])
            self.act(bm, bm, AF.Exp, scale=-1.0, R=[], W=[tbm])
            self.tt("pool", B["hQt"][:, ct, :], q, Ep, ALU.mult, R=[tq, tEp], Wadd=[T["hQt"]])
            self.tt("dve", B["hKt"][:, ct, :], k, bm, ALU.mult, R=[tk, tbm], Wadd=[T["hKt"]])
            self.tt("pool", B["hKlo"][:, ct, :], B["hKt"][:, ct, :], c["mlo"], ALU.mult, R=[T["hKt"], tcs], Wadd=[T["hKlo"]])
            self.cp("dve", B["hgc"][:, ct, 1, :], Ep.rearrange("p (c t) -> p c t", t=64)[:, :, 63], R=[tEp], Wadd=[T["hgc"]])
            bg_, phg, tphg = self.proj(1376 + ct * 128, 128)
            self.act(B["hgb"][:, ct, :], phg[:, 0:BLK], AF.Silu, R=[tphg], Wadd=[T["hgb"]])
            fw.release(bg_)
            self.tmp_release(m2)
        if STAGE < 5.1:
            self.tmp_release(mark); return
        if os.environ.get("KBAR3"):
            self.barrier()
        for t in (range(NTB) if "hv" not in os.environ.get("KSKIP", "") else []):
            bi_, pv, tpv = fw.bank()
            for k_ in range(8):
                self.mm(pv[:, 0:256], B["hT"][:, k_, t * 128:(t + 1) * 128], B["w_in"][:, k_, 1120:1376], start=(k_ == 0), stop=(k_ == 7),
                        R=[T["hT"], T["w_in"]], W=[tpv])
            self.cp("dve" if "hvdve" in os.environ.get("KSKIP", "") else "act", B["hv"][:, t, :], pv[:, 0:256], R=[tpv], Wadd=[T["hv"]])
        for t in (range(NTB) if "hkt" not in os.environ.get("KSKIP", "") else []):
            bi_, pb, tpb = fw.bank()
            pbb = pb[:].bitcast(BF16)
            for ct in range(2):
                self.tr(pbb[:, ct * 128:(ct + 1) * 128], B["hKt"][:, ct, t * 128:(t + 1) * 128], c["idb"], R=[T["hKt"], tcs], W=[tpb])
            self.cp("dve", B["hKtT"][:, t, :], pbb[:, 0:256], R=[tpb], Wadd=[T["hKtT"]])
        if STAGE < 5.2:
            self.tmp_release(mark); return
        pAs = [fw.bank(hold=True) for _ in range(2)]
        for ch in range(NC6):
            for h in range(4):
                ct, r0, hp = h // 2, 64 * (h % 2), h % 2
                q4 = 64 * (ch % 2)
                sl = (ch // 2) * 2 + ct
                pa_, pA, tpA = pAs[hp]
                self.mm(pA[q4:q4 + 64, sl * 64:sl * 64 + 32], B["hKlo"][r0:r0 + 64, ct, ch * 64:(ch + 1) * 64],
                        B["hQt"][r0:r0 + 64, ct, ch * 64:ch * 64 + 32], R=[T["hKlo"], T["hQt"]], W=[tpA])
                self.mm(pA[q4:q4 + 64, sl * 64 + 32:(sl + 1) * 64], B["hKt"][r0:r0 + 64, ct, ch * 64:(ch + 1) * 64],
                        B["hQt"][r0:r0 + 64, ct, ch * 64 + 32:(ch + 1) * 64], R=[T["hKt"], T["hQt"]], W=[tpA])
        for hp in range(2):
            pa_, pA, tpA = pAs[hp]
            for cp_ in range(2):
                q4 = 64 * cp_
                dst = B["hAm"][q4:q4 + 64, :, :, q4:q4 + 64].rearrange("p t (c h) s -> p t c h s", h=2)[:, :, :, hp, :]
                src = pA[q4:q4 + 64, 0:NTB * 128].rearrange("p (t c s) -> p t c s", c=2, s=64)
                self.tt("dve", dst, src, c["hmask"][q4:q4 + 64, :].unsqueeze(1).unsqueeze(1).to_broadcast([64, NTB, 2, 64]), ALU.mult,
                        R=[tpA, tcs], Wadd=[T["hAm"]])
            fw.release(pa_)
        if STAGE < 5.3:
            self.tmp_release(mark); return
        pSs = [fw.bank(hold=True) for _ in range(2)]
        for ch in range(NC6):
            q4 = 64 * (ch % 2)
            tt_ = ch // 2
            ps_, pS, tpS = pSs[ch % 2]
            for h in range(4):
                ct, r0 = h // 2, 64 * (h % 2)
                self.mm(pS[r0:r0 + 64, (tt_ * 2 + ct) * 64:(tt_ * 2 + ct + 1) * 64],
                        B["hKtT"][q4:q4 + 64, tt_, ct * 128 + r0:ct * 128 + r0 + 64], B["hv"][q4:q4 + 64, tt_, h * 64:(h + 1) * 64],
                        R=[T["hKtT"], T["hv"]], W=[tpS])
        for ch in range(NC6):
            tt_ = ch // 2
            ps_, pS, tpS = pSs[ch % 2]
            for ct in range(2):
                self.ts("dve", B["hSp"][:, ct, :], B["hS"][:, ct, :], B["hgc"][:, ct, 0, ch:ch + 1], None, ALU.mult, R=[T["hS"], T["hgc"]], W=[T["hSp"]])
                self.tt("dve", B["hS"][:, ct, :], pS[:, (tt_ * 2 + ct) * 64:(tt_ * 2 + ct + 1) * 64], B["hSp"][:, ct, :], ALU.add, R=[tpS, T["hSp"]], W=[T["hS"]])
                self.ts("dve", B["hS"][:, ct, :], B["hS"][:, ct, :], B["hgc"][:, ct, 1, ch:ch + 1], None, ALU.mult, R=[T["hgc"]], W=[T["hS"]])
            self.cp("act", B["hSb"][0:64, :, ch, 0:64], B["hSp"][0:64, :, :], R=[T["hSp"]], Wadd=[T["hSb"]])
            self.cp("act", B["hSb"][64:128, :, ch, 64:128], B["hSp"][64:128, :, :], R=[T["hSp"]], Wadd=[T["hSb"]])
        for ps_, pS, tpS in pSs:
            fw.release(ps_)
        if STAGE < 5.4:
            self.tmp_release(mark); return
        pos_ = [fw.bank(hold=True) for _ in range(2)]
        for ct in range(2):
            po_, po, tpo = pos_[ct]
            for tt_ in range(NTB):
                for hh in range(2):
                    h = ct * 2 + hh
                    r0 = 64 * hh
                    self.mm(po[r0:r0 + 64, tt_ * 128:(tt_ + 1) * 128], B["hv"][:, tt_, h * 64:(h + 1) * 64], B["hAm"][:, tt_, h, :],
                            start=True, stop=True, R=[T["hv"], T["hAm"]], W=[tpo])
        pis_ = [fw.bank(hold=True) for _ in range(2)]
        for ct in range(2):
            pi_, pi, tpi = pis_[ct]
            for ch in range(NC6):
                self.mm(pi[:, ch * 64:(ch + 1) * 64], B["hSb"][:, ct, ch, :], B["hQt"][:, ct, ch * 64:(ch + 1) * 64],
                        start=True, stop=True, R=[T["hSb"], T["hQt"]], W=[tpi])
        osum = []
        for ct in range(2):
            po_, po, tpo = pos_[ct]
            pi_, pi, tpi = pis_[ct]
            oi, toi = self.tmp("F")
            self.cp("dve", oi, pi[:, 0:BLK], R=[tpi], W=[toi])
            self.tt("dve", oi, po[:, 0:BLK], oi, ALU.add, R=[tpo], W=[toi])
            osum.append((oi, toi))
            fw.release(po_); fw.release(pi_)
        for ct in range(2):
            oi, toi = osum[ct]
            o2, to2 = self.tmp("H")
            self.act(o2, oi, AF.Square, R=[toi], W=[to2])
            bi_, pn, tpn = fw.bank()
            self.mm(pn[:, 0:BLK], c["bd64"], o2, R=[tcs, to2], W=[tpn])
            rs, trs = self.tmp("F")
            self.rstd_from_sum(rs, pn[:, 0:BLK], 64, R=[tpn], W=[trs])
            self.stt(oi, oi, vT[:, 25:26], rs, ALU.mult, ALU.mult, R=[tvT, trs], W=[toi])
            self.tt("pool", B["yT"][:, 4 + ct, :], oi, B["hgb"][:, ct, :], ALU.mult, R=[toi, T["hgb"]], Wadd=[T["yT"]])
        self.tmp_release(mark)


    def mix_conv(self, l, bi):
        fw, B, T, c, d = self.fw, self.B, self.T, self.c, self.d
        tcs = self.t_const
        NTB = BLK // 128
        NCH = BLK // 32
        t0 = bi * BLK
        xsrc = d["x_in"] if l == 0 else self.out
        vT, tvT = B["vT"], T["vT"]
        st, tst = B["st"], T["st"]
        hc, thc = B["hcol"], T["hcol"]
        mark = self.tmp_mark()
        for ct in range(2):
            ba_, pa, tpa = self.proj(1632 + ct * 128, 128)
            bg_, pg, tpg = self.proj(1888 + ct * 128, 128)
            sg, tsg = self.tmp("F")
            self.act(sg, pg[:, 0:BLK], AF.Sigmoid, R=[tpg], W=[tsg])
            self.tt("dve", B["cu"][:, ct, 32:32 + BLK], pa[:, 0:BLK], sg, ALU.mult, R=[tpa, tsg], Wadd=[T["cu"]])
            fw.release(ba_); fw.release(bg_)
        cvs = []
        for ct in range(2):
            bi_, pc, tpc = fw.bank()
            for j in range(31):
                self.mm(pc[:, 0:BLK], B["dgw"][:, ct, j, :], B["cu"][:, ct, 2 + j:2 + j + BLK], start=(j == 0), stop=(j == 30),
                        R=[T["dgw"], T["cu"]], W=[tpc])
            cv, tcv = self.tmp("F")
            self.act(cv, pc[:, 0:BLK], AF.Identity, bias=vT[:, 26 + ct:27 + ct], R=[tpc, tvT], W=[tcv])
            cvs.append((cv, tcv))
        self.cp("pool", B["cu"][:, :, 2:32], B["cu"][:, :, BLK + 2:BLK + 32], R=[], W=[T["cu"]])
        bm_, pm, tpm = fw.bank(hold=True)
        bm2_, pm2, tpm2 = fw.bank(hold=True)
        for ct in range(2):
            cv, tcv = cvs[ct]
            self.mm(pm[:, 0:BLK], c["ones_f"], cv, start=(ct == 0), stop=(ct == 1), R=[tcs, tcv], W=[tpm])
        sq, tsq = self.tmp("F")
        for ct in range(2):
            cv, tcv = cvs[ct]
            self.act(sq, cv, AF.Square, R=[tcv], W=[tsq])
            self.mm(pm2[:, 0:BLK], c["ones_f"], sq, start=(ct == 0), stop=(ct == 1), R=[tcs, tsq], W=[tpm2])
        mu, tmu = self.tmp("F")
        var, tvar = self.tmp("F")
        self.ts("dve", mu, pm[:, 0:BLK], 1.0 / 256, None, ALU.mult, R=[tpm], W=[tmu])
        self.stt(var, mu, -1.0, mu, ALU.mult, ALU.mult, R=[tmu], W=[tvar])
        self.stt(var, pm2[:, 0:BLK], 1.0 / 256, var, ALU.mult, ALU.add, R=[tpm2], W=[tvar])
        fw.release(bm_); fw.release(bm2_)
        self.rstd_from_sum(var, var, 1.0, R=[], W=[tvar])
        for ct in range(2):
            cv, tcv = cvs[ct]
            self.tt("pool", cv, cv, mu, ALU.subtract, R=[tmu], W=[tcv])
            self.stt(cv, cv, vT[:, 28 + ct:29 + ct], var, ALU.mult, ALU.mult, R=[tvT, tvar], W=[tcv])
            self.act(B["yT"][:, 6 + ct, :], cv, AF.Silu, bias=vT[:, 30 + ct:31 + ct], R=[tcv, tvT], Wadd=[T["yT"]])
        self.tmp_release(mark)

    def mix_pool(self, l, bi):
        fw, B, T, c, d = self.fw, self.B, self.T, self.c, self.d
        tcs = self.t_const
        NTB = BLK // 128
        NCH = BLK // 32
        t0 = bi * BLK
        xsrc = d["x_in"] if l == 0 else self.out
        vT, tvT = B["vT"], T["vT"]
        st, tst = B["st"], T["st"]
        hc, thc = B["hcol"], T["hcol"]
        for t in range(NTB):
            bi_, pp, tpp = fw.bank()
            for k_ in range(8):
                self.mm(pp[:, 0:256], B["hT"][:, k_, t * 128:(t + 1) * 128], B["w_in"][:, k_, 352:608], start=(k_ == 0), stop=(k_ == 7),
                        R=[T["hT"], T["w_in"]], W=[tpp])
            self.cp("act", B["pu"][:, 1 + t, :], pp[:, 0:256], R=[tpp], Wadd=[T["pu"]])
        for t in range(NTB):
            first = (bi == 0 and t == 0)
            mark = self.tmp_mark()
            bi_, pq, tpq = fw.bank()
            for g in range(4):
                r0, sl = 64 * (g % 2), g // 2
                o_ = pq[r0:r0 + 64, sl * 128:(sl + 1) * 128]
                self.mm(o_, B["pu"][:, 1 + t, g * 64:(g + 1) * 64], (c["B0"] if first else c["Bmain"])[g], start=True, stop=first,
                        R=[T["pu"], tcs], W=[tpq])
                if not first:
                    self.mm(o_, B["pu"][:, t, g * 64:(g + 1) * 64], c["Bprev"][g], start=False, stop=True, R=[T["pu"], tcs], W=[tpq])
            pl, tpl = self.tmp("H")
            self.cp("dve", pl[:, 0:256], pq[:, 0:256], R=[tpq], W=[tpl])
            bi_, py, tpy = fw.bank()
            for sl in range(2):
                self.mm(py[:, sl * 128:(sl + 1) * 128], B["wpl"][:, sl, :], pl[:, sl * 128:(sl + 1) * 128], R=[T["wpl"], tpl], W=[tpy])
            for sl in range(2):
                self.ts("dve", B["yT"][:, 2 + sl, t * 128:(t + 1) * 128], py[:, sl * 128:(sl + 1) * 128], vT[:, 23 + sl:24 + sl], None, ALU.mult,
                        R=[tpy, tvT], Wadd=[T["yT"]])
            self.tmp_release(mark)
        self.cp("pool", B["pu"][:, 0, :], B["pu"][:, NTB, :], R=[], W=[T["pu"]])

    def mix_dbg(self, l, bi):
        fw, B, T, c, d = self.fw, self.B, self.T, self.c, self.d
        tcs = self.t_const
        NTB = BLK // 128
        NCH = BLK // 32
        t0 = bi * BLK
        xsrc = d["x_in"] if l == 0 else self.out
        vT, tvT = B["vT"], T["vT"]
        st, tst = B["st"], T["st"]
        hc, thc = B["hcol"], T["hcol"]
        if self.dbg and d["dbg_y"] is not None and l == 0:
            for kx in range(8):
                mk2 = self.tmp_mark()
                f_, tf_ = self.tmp("F")
                self.cp("dve", f_, B["yT"][:, kx, :], R=[T["yT"]], W=[tf_])
                fw.dma("sp", d["dbg_y"][kx * 128:(kx + 1) * 128, t0:t0 + BLK], f_, R=[tf_], Wadd=[self.t["dbg"]])
                self.tmp_release(mk2)

    def mix_out(self, l, bi):
        fw, B, T, c, d = self.fw, self.B, self.T, self.c, self.d
        tcs = self.t_const
        NTB = BLK // 128
        NCH = BLK // 32
        t0 = bi * BLK
        xsrc = d["x_in"] if l == 0 else self.out
        vT, tvT = B["vT"], T["vT"]
        st, tst = B["st"], T["st"]
        hc, thc = B["hcol"], T["hcol"]
        for t in range(NTB):
            tile = bi * NTB + t
            rows = slice(t0 + t * 128, t0 + (t + 1) * 128)
            mark = self.tmp_mark()
            xo, txo = self.tmp("Dd")
            fw.dma("sp", xo, xsrc[rows, :], R=[self.t["xd"][tile]], W=[txo])
            for half in range(2):
                bi_, po, tpo = fw.bank()
                for k_ in range(8):
                    self.mm(po[:, :], B["yT"][:, k_, t * 128:(t + 1) * 128], B["w_out"][:, k_, half * 512:(half + 1) * 512],
                            start=(k_ == 0), stop=(k_ == 7), R=[T["yT"], T["w_out"]], W=[tpo])
                self.tt("dve", xo[:, half * 512:(half + 1) * 512], po[:, :], xo[:, half * 512:(half + 1) * 512], ALU.add, R=[tpo], W=[txo])
            fw.dma("sp", self.out[rows, :], xo, R=[txo], W=[self.t["xd"][tile]])
            self.act(B["junk"], xo, AF.Square, R=[txo], W=[T["junk"]])
            fw.op("dve", lambda e, t=t: e.tensor_reduce(out=st[:, 8 + t:9 + t], in_=B["junk"], axis=mybir.AxisListType.X, op=ALU.add),
                  R=[T["junk"]], W=[tst])
            self.rstd_from_sum(st[:, 12 + t:13 + t], st[:, 8 + t:9 + t], D, R=[tst], W=[tst])
            h2, th2 = self.tmp("Dd")
            self.stt(h2, xo, st[:, 12 + t:13 + t], B["G2row"], ALU.mult, ALU.mult, R=[txo, tst, T["G2row"]], W=[th2])
            self.tt("pool", h2, h2, B["sh2row"], ALU.add, R=[T["sh2row"]], W=[th2])
            hb_, thb = B["h2b"][t % 2], T["h2b"][t % 2]
            self.cp("act", hb_, h2, R=[th2], W=[thb])
            fw.dma("sp", d["h2d"][rows, :], hb_, R=[thb], Wadd=[self.t["h2d"]])
            for kk in range(2):
                bi_, pb, tpb = fw.bank()
                for k4 in range(4):
                    kx = kk * 4 + k4
                    self.tr(pb[:, k4 * 128:(k4 + 1) * 128], h2[:, kx * 128:(kx + 1) * 128], c["idf"], R=[th2, tcs], W=[tpb])
                self.cp("dve" if kk else "act", B["h2T"][:, kk * 4:(kk + 1) * 4, :].rearrange("p a b -> p (a b)"), pb[:, :], R=[tpb], Wadd=[T["h2T"]])
            bi_, plg, tplg = fw.bank()
            for k_ in range(8):
                self.mm(plg[:, 0:36], B["h2T"][:, k_, :], B["rwf"][:, k_, :], start=(k_ == 0), stop=(k_ == 7), R=[T["h2T"], T["rwf"]], W=[tplg])
            lg, tlg = B["lg"], T["lg"]
            self.tt("dve", lg, plg[:, 0:36], B["rbb"], ALU.add, R=[tplg, T["rbb"]], W=[tlg])
            self.route_tile(tile)
            self.tmp_release(mark)


    def route_tile(self, tile):
        fw, B, T, c = self.fw, self.B, self.T, self.c
        lg, tlg = B["lg"], T["lg"]
        rt, trt = B["rt"], T["rt"]
        V = lambda a, b: rt[:, a:b]
        RW = dict(R=[tlg], W=[trt])
        fw.op("dve", lambda e: e.tensor_reduce(out=V(0, 1), in_=lg[:, 0:4], axis=mybir.AxisListType.X, op=ALU.max), **RW)
        self.ts("dve", V(4, 8), lg[:, 0:4], V(0, 1), None, ALU.is_equal, **RW)
        self.ts("dve", V(1, 2), V(0, 1), -1.0, None, ALU.mult, **RW)
        self.act(V(8, 12), lg[:, 0:4], AF.Exp, bias=V(1, 2), **RW)
        fw.op("dve", lambda e: e.tensor_reduce(out=V(2, 3), in_=V(8, 12), axis=mybir.AxisListType.X, op=ALU.add), **RW)
        fw.op("dve", lambda e: e.reciprocal(out=V(3, 4), in_=V(2, 3)), **RW)
        self.ts("dve", V(16, 24), lg[:, 4:12], V(4, 5), None, ALU.mult, **RW)
        for g in range(1, 4):
            self.stt(V(16, 24), lg[:, 4 + 8 * g:12 + 8 * g], V(4 + g, 5 + g), V(16, 24), ALU.mult, ALU.add, **RW)
        fw.op("dve", lambda e: e.max(out=V(24, 32), in_=V(16, 24)), **RW)
        self.ts("dve", V(32, 40), V(16, 24), V(24, 25), None, ALU.is_equal, **RW)
        self.ts("dve", V(40, 48), V(16, 24), V(25, 26), None, ALU.is_equal, **RW)
        self.tt("dve", V(12, 13), V(25, 26), V(24, 25), ALU.subtract, **RW)
        self.act(V(13, 14), V(12, 13), AF.Exp, **RW)
        self.ts("dve", V(14, 15), V(13, 14), 1.0, None, ALU.add, **RW)
        fw.op("dve", lambda e: e.reciprocal(out=V(14, 15), in_=V(14, 15)), **RW)
        self.tt("dve", V(15, 16), V(13, 14), V(14, 15), ALU.mult, **RW)
        self.tt("dve", B["pw"][:, tile, 0:1], V(14, 15), V(3, 4), ALU.mult, R=[trt], Wadd=[T["pw"]])
        self.tt("dve", B["pw"][:, tile, 1:2], V(15, 16), V(3, 4), ALU.mult, R=[trt], Wadd=[T["pw"]])
        for g in range(4):
            self.ts("dve", B["A1"][:, tile, g * 8:(g + 1) * 8], V(32, 40), V(4 + g, 5 + g), None, ALU.mult, R=[trt], Wadd=[T["A1"]])
            self.ts("dve", B["A2"][:, tile, g * 8:(g + 1) * 8], V(40, 48), V(4 + g, 5 + g), None, ALU.mult, R=[trt], Wadd=[T["A2"]])
        Ab, tAb = B["Ab"][tile % 2], T["Ab"][tile % 2]
        self.tt("dve", Ab, B["A1"][:, tile, :], B["A2"][:, tile, :], ALU.add, R=[T["A1"], T["A2"]], W=[tAb])
        bi_, pr, tpr = fw.bank()
        self.mm(pr[:, 0:32], c["ustrict"], Ab, R=[self.t_const, tAb], W=[tpr])
        self.mm(pr[:, 32:64], c["ones_b"], Ab, R=[self.t_const, tAb], W=[tpr])
        self.tt("dve", V(48, 80), pr[:, 0:32], B["carry"], ALU.add, R=[tpr, T["carry"]], W=[trt])
        self.tt("dve", V(16, 48), V(48, 80), B["A1"][:, tile, :], ALU.mult, R=[T["A1"]], W=[trt])
        fw.op("dve", lambda e: e.tensor_reduce(out=B["r12"][:, tile, 0:1], in_=V(16, 48), axis=mybir.AxisListType.X, op=ALU.add), R=[trt], Wadd=[T["r12"]])
        self.tt("dve", V(16, 48), V(48, 80), B["A2"][:, tile, :], ALU.mult, R=[T["A2"]], W=[trt])
        fw.op("dve", lambda e: e.tensor_reduce(out=B["r12"][:, tile, 1:2], in_=V(16, 48), axis=mybir.AxisListType.X, op=ALU.add), R=[trt], Wadd=[T["r12"]])
        self.tt("dve", B["carry"], B["carry"], pr[:, 32:64], ALU.add, R=[tpr], W=[T["carry"]])

    def moe(self, l):
        fw, B, T, c, d = self.fw, self.B, self.T, self.c, self.d
        tcs = self.t_const
        NT, NBK = self.NT, self.NBLKS
        BIG = float(self.L * NEXP * 128)
        tM = Tok("moe_small")
        self.ts("dve", B["cnt_i"], B["carry"], 127.0, None, ALU.add, R=[T["carry"]], W=[tM])
        fw.op("dve", lambda e: e.tensor_single_scalar(out=B["cnt_i"], in_=B["cnt_i"], scalar=7, op=ALU.arith_shift_right), R=[tM], W=[tM])
        fw.op("dve", lambda e: e.tensor_single_scalar(out=B["cnt_i"], in_=B["cnt_i"], scalar=7, op=ALU.logical_shift_left), R=[tM], W=[tM])
        self.cp("dve", B["pad_f"], B["cnt_i"], R=[tM], W=[tM])
        fw.op("dve", lambda e: e.tensor_tensor_scan(out=B["pend"], data0=c["ones_f"][:, 0:32], data1=B["pad_f"], initial=0.0,
                                                    op0=ALU.mult, op1=ALU.add), R=[tM, tcs], W=[tM])
        self.tt("dve", B["base"], B["pend"], B["pad_f"], ALU.subtract, R=[tM], W=[tM])
        self.ts("dve", B["bexp"], B["bst"], B["pend"][:, 0:1], None, ALU.is_ge, R=[tM, T["bst"]], W=[tM])
        for e_ in range(1, NEXP):
            self.stt(B["bexp"], B["bst"], B["pend"][:, e_:e_ + 1], B["bexp"], ALU.is_ge, ALU.add, R=[tM], W=[tM])
        self.ts("dve", B["bexp"], B["bexp"], float(NEXP - 1), None, ALU.min, R=[tM], W=[tM])
        fw.op("pool", lambda e: e.memset(B["bchg"][:, 0:1], 1.0), R=[tM], W=[tM])
        self.tt("dve", B["bchg"][:, 1:NBK], B["bexp"][:, 1:NBK], B["bexp"][:, 0:NBK - 1], ALU.not_equal, R=[tM], W=[tM])
        self.ts("dve", B["btmp"], B["bexp"], 128.0, B["pidx"][:, 0:1], ALU.mult, ALU.add, R=[tM, T["pidx"]], W=[tM])
        self.ts("dve", B["btmp"], B["btmp"], float(l * NEXP * 128), None, ALU.add, R=[tM], W=[tM])
        if os.environ.get("KSKIPW", "0") == "1":
            self.tt("dve", B["btmp"], B["btmp"], B["bchg"], ALU.mult, R=[tM], W=[tM])
            self.ts("dve", B["bchg"], B["bchg"], -BIG, BIG, ALU.mult, ALU.add, R=[tM], W=[tM])
            self.tt("dve", B["btmp"], B["btmp"], B["bchg"], ALU.add, R=[tM], W=[tM])
        self.cp("dve", B["widx"], B["btmp"], R=[tM], W=[tM])
        KM = int(os.environ.get("KMOE", "99"))
        for tile in range(NT):
            for k_ in range(2):
                t32, tt32 = B["t32"][k_], T["t32"][k_]
                self.tt("dve", t32, (B["A1"] if k_ == 0 else B["A2"])[:, tile, :], B["base"], ALU.mult, R=[tM, T["A1"], T["A2"]], W=[tt32])
                fw.op("dve", lambda e, t32=t32, tile=tile, k_=k_: e.tensor_reduce(out=B["slot"][:, tile, k_:k_ + 1], in_=t32, axis=mybir.AxisListType.X, op=ALU.add),
                      R=[tt32], Wadd=[tM])
        self.tt("dve", B["slot"], B["slot"], B["r12"], ALU.add, R=[tM, T["r12"]], W=[tM])
        self.cp("dve", B["sloti"], B["slot"], R=[tM], W=[tM])
        if KM < 2:
            return
        ts_ = self.t["slotd"]
        tzz = Tok()
        fw.op("pool", lambda e: e.memset(B["zz"], 0), W=[tzz])
        sv = d["slotd"].rearrange("(p j) o -> p (j o)", p=128)
        ncol = NBK * 16
        fw.sync("sp", W=[ts_])
        for c0 in range(0, ncol, 1024):
            c1 = min(ncol, c0 + 1024)
            fw.dma("sp", sv[:, c0:c1], B["zz"][:, 0:c1 - c0], R=[tzz], Wadd=[ts_])
        fw.sync("pool", R=[ts_])
        for tile in range(NT):
            tk, ttk = B["tk16"][tile % 2], T["tk16"][tile % 2]
            fw.op("pool", lambda e, tk=tk, tile=tile: e.iota(tk, pattern=[[0, 16]], base=tile * 128, channel_multiplier=1), W=[ttk])
            for k_ in range(2):
                fw.dma("pool", None, None, R=[tM, ttk], Wadd=[ts_],
                       fn=lambda e, tile=tile, k_=k_, tk=tk: e.indirect_dma_start(
                           out=d["slotd"][:, :], out_offset=bass.IndirectOffsetOnAxis(ap=B["sloti"][:, tile, k_:k_ + 1], axis=0),
                           in_=tk, in_offset=None))
        if KM < 3:
            return
        w1v = d["w1r"].rearrange("r (a n) -> r a n", n=2048)
        w3v = d["w3r"].rearrange("r (a n) -> r a n", n=2048)
        w2v = d["w2r"].rearrange("r (a n) -> r a n", n=2048)
        tys = self.t["ysd"]
        fw.sync("sp", W=[tys])
        for j in range(NBK):
            b2 = j % 2
            tki, ttki = B["tki"][b2], T["tki"][b2]
            fw.dma("sp", tki, d["slotd"][j * 128:(j + 1) * 128, :], R=[ts_], W=[ttki])
            xb, txb = B["xb"][b2], T["xb"][b2]
            fw.dma("pool", None, None, R=[ttki, self.t["h2d"]], W=[txb],
                   fn=lambda e, tki=tki, xb=xb: e.indirect_dma_start(
                       out=xb, out_offset=None, in_=d["h2d"][:, :], in_offset=bass.IndirectOffsetOnAxis(ap=tki[:, 0:1], axis=0)))
            if KM < 4:
                continue
            for nm, wv in (("w1", d["w1r"]), ("w3", d["w3r"]), ("w2", d["w2r"])):
                fw.dma("pool", None, None, R=[tM], W=[T["wst"]],
                       fn=lambda e, wv=wv, j=j: e.indirect_dma_start(
                           out=B["wst"], out_offset=None, in_=wv,
                           in_offset=bass.IndirectOffsetOnAxis(ap=B["widx"][:, j:j + 1], axis=0)))
                dstf = B[nm].rearrange("p a n -> p (a n)")
                fw.sync("act", W=[T[nm]]); fw.sync("dve", W=[T[nm]])
                self.cp("act", dstf[:, 0:2048], B["wst"][:, 0:2048], R=[T["wst"]], Wadd=[T[nm]])
                self.cp("dve", dstf[:, 2048:4096], B["wst"][:, 2048:4096], R=[T["wst"]], Wadd=[T[nm]])
            if KM < 5:
                continue
            bi_, pb, tpb = fw.bank()
            pbb = pb[:].bitcast(BF16)
            for k_ in range(8):
                self.tr(pbb[:, k_ * 128:(k_ + 1) * 128], xb[:, k_ * 128:(k_ + 1) * 128], c["idb"], R=[txb, tcs], W=[tpb])
            xbT, txbT = B["xbT"][b2], T["xbT"][b2]
            self.cp("dve", xbT.rearrange("p a b -> p (a b)"), pbb[:, 0:1024], R=[tpb], W=[txbT])
            b1_, p1, tp1 = fw.bank(hold=True)
            b3_, p3, tp3 = fw.bank(hold=True)
            for ec in range(4):
                for k_ in range(8):
                    self.mm(p1[:, ec * 128:(ec + 1) * 128], B["w1"][:, k_, ec * 128:(ec + 1) * 128], xbT[:, k_, :], start=(k_ == 0), stop=(k_ == 7),
                            R=[T["w1"], txbT], W=[tp1])
            for ec in range(4):
                for k_ in range(8):
                    self.mm(p3[:, ec * 128:(ec + 1) * 128], B["w3"][:, k_, ec * 128:(ec + 1) * 128], xbT[:, k_, :], start=(k_ == 0), stop=(k_ == 7),
                            R=[T["w3"], txbT], W=[tp3])
            self.act(B["h1"].rearrange("p a b -> p (a b)"), p1[:, :], AF.Silu, R=[tp1], W=[T["h1"]])
            hbT, thbT = B["hbT"][b2], T["hbT"][b2]
            self.tt("dve", hbT.rearrange("p a b -> p (a b)"), p3[:, :], B["h1"].rearrange("p a b -> p (a b)"), ALU.mult, R=[tp3, T["h1"]], W=[thbT])
            fw.release(b1_); fw.release(b3_)
            yb, tyb = B["yb"][b2], T["yb"][b2]
            for half in range(2):
                bi_, py, tpy = fw.bank()
                for ec in range(4):
                    self.mm(py[:, :], hbT[:, ec, :], B["w2"][:, ec, half * 512:(half + 1) * 512], start=(ec == 0), stop=(ec == 3),
                            R=[thbT, T["w2"]], W=[tpy])
                self.cp("act" if half else "dve", yb[:, half * 512:(half + 1) * 512], py[:, :], R=[tpy], Wadd=[tyb])
            fw.dma("sp", d["ysd"][j * 128:(j + 1) * 128, :], yb, R=[tyb], Wadd=[tys])
        if KM < 6:
            return
        for tile in range(NT):
            b2 = tile % 2
            rows = slice(tile * 128, (tile + 1) * 128)
            y1, ty1 = B["y1"][b2], T["y1"][b2]
            y2, ty2 = B["y2"][b2], T["y2"][b2]
            xm, txm = B["xm"][b2], T["xm"][b2]
            for yy, tyy, k_ in ((y1, ty1, 0), (y2, ty2, 1)):
                fw.dma("pool", None, None, R=[tM, tys], W=[tyy],
                       fn=lambda e, yy=yy, k_=k_, tile=tile: e.indirect_dma_start(
                           out=yy, out_offset=None, in_=d["ysd"][:, :], in_offset=bass.IndirectOffsetOnAxis(ap=B["sloti"][:, tile, k_:k_ + 1], axis=0)))
            fw.dma("sp", xm, self.out[rows, :], R=[self.t["xd"][tile]], W=[txm])
            self.ts("dve", y1, y1, B["pw"][:, tile, 0:1], None, ALU.mult, R=[T["pw"]], W=[ty1])
            self.stt(y1, y2, B["pw"][:, tile, 1:2], y1, ALU.mult, ALU.add, R=[ty2, T["pw"]], W=[ty1])
            self.tt("pool", y1, y1, B["g2row"], ALU.mult, R=[T["g2row"]], W=[ty1])
            self.tt("dve", xm, xm, y1, ALU.add, R=[ty1], W=[txm])
            fw.dma("sp", self.out[rows, :], xm, R=[txm], W=[self.t["xd"][tile]])


_NC_CACHE = {}


def _get_nc(S, L, dbg=False):
    key = (S, L, dbg)
    if key not in _NC_CACHE:
        b = Builder(S, L, dbg)
        b.build()
        _NC_CACHE[key] = b
    return _NC_CACHE[key]


def _prep_shared(inp, L):
    f = lambda a: np.ascontiguousarray(np.asarray(a, dtype=np.float32))
    vecs = np.zeros((L, 96, 128), np.float32)
    n1, n2 = f(inp["norm1_g"]), f(inp["norm2_g"])
    vecs[:, 0:8] = n1.reshape(L, 8, 128)
    vecs[:, 8:16] = n2.reshape(L, 8, 128)
    qa = f(inp["q_a_norm_g"])
    vecs[:, 16] = qa[:, 0:128]
    vecs[:, 17, 0:64] = qa[:, 128:192]
    vecs[:, 18] = f(inp["kv_a_norm_g"])
    for col, nm in ((19, "q_norm_g"), (21, "k_norm_g")):
        g = f(inp[nm])
        vecs[:, col, 0:96] = g
        vecs[:, col + 1, 0:64] = g[:, 0:64]
        vecs[:, col + 1, 64:80] = g[:, 80:96]
        vecs[:, col + 1, 80:96] = g[:, 64:80]
    vecs[:, 23:25] = f(inp["pool_scale"]).reshape(L, 2, 128)
    og = f(inp["hgrn_out_norm_g"])
    vecs[:, 25, 0:64] = og
    vecs[:, 25, 64:128] = og
    vecs[:, 26:28] = f(inp["conv_dw_b"]).reshape(L, 2, 128)
    vecs[:, 28:30] = f(inp["conv_ln_g"]).reshape(L, 2, 128)
    vecs[:, 30:32] = f(inp["conv_ln_b"]).reshape(L, 2, 128)
    vecs[:, 32:94] = f(inp["conv_dw_w"]).reshape(L, 31, 2, 128).reshape(L, 62, 128)
    cst = np.zeros((128, 8), np.float32)
    half = 16
    inv_freq = (np.float32(10000.0) ** (-np.arange(half, dtype=np.float32) / np.float32(half))).astype(np.float32)
    cst[64:80, 0] = inv_freq
    cst[80:96, 0] = inv_freq
    cst[64:80, 1] = -1.0
    cst[80:96, 1] = 1.0
    rows = np.zeros((L, 2, D), np.float32)
    rows[:, 0] = n2
    w1 = f(inp["w1"]); w3 = f(inp["w3"]); w2 = f(inp["w2"])
    sh = dict(
        cst=cst, vecs=vecs, rows=rows,
        w_ada=f(inp["w_ada"]), b_ada=f(inp["b_ada"]),
        lbl=f(inp["hgrn_lb_logits"]).reshape(2 * L, 128),
        w_in=f(inp["w_in"]), w_uq=f(inp["w_uq"]), w_ukv=f(inp["w_ukv"]), w_pool=f(inp["w_pool"]), w_out=f(inp["w_out"]),
        rw=np.ascontiguousarray(np.concatenate([f(inp["router_group_w"]), f(inp["router_expert_w"])], axis=2)),
        rb=np.ascontiguousarray(np.concatenate([f(inp["router_group_b"]), f(inp["router_expert_b"])], axis=1)),
        w1r=np.ascontiguousarray(w1.reshape(L, NEXP, 8, 128, DEXP).transpose(0, 1, 3, 2, 4)).reshape(L * NEXP * 128, 8 * DEXP),
        w3r=np.ascontiguousarray(w3.reshape(L, NEXP, 8, 128, DEXP).transpose(0, 1, 3, 2, 4)).reshape(L * NEXP * 128, 8 * DEXP),
        w2r=np.ascontiguousarray(w2.reshape(L, NEXP, 4, 128, D).transpose(0, 1, 3, 2, 4)).reshape(L * NEXP * 128, 4 * D),
    )
    return sh


def run_model(inp, dbg=False):
    x = np.asarray(inp["x"], dtype=np.float32)
    Bn, S, _ = x.shape
    L = int(np.asarray(inp["w_ada"]).shape[0])
    b = _get_nc(S, L, dbg)
    sh = _prep_shared(inp, L)
    c = np.asarray(inp["c"], dtype=np.float32)
    pos = np.asarray(inp["positions"]).astype(np.int32)
    in_maps = []
    for i in range(Bn):
        m = dict(sh)
        m["x"] = np.ascontiguousarray(x[i])
        m["c"] = np.ascontiguousarray(c[i].reshape(8, 128))
        m["pos"] = np.ascontiguousarray(pos[i].reshape(1, S))
        in_maps.append(m)
    res = run_bass_kernel_spmd(b.nc, in_maps, core_ids=list(range(Bn)))
    out = np.stack([np.asarray(r["out"]) for r in res.results], axis=0).astype(np.float32)
    if dbg:
        return out, [np.asarray(r["dbg_y"]) for r in res.results]
    return out


def kernel(**inputs):
    return run_model(inputs)
```
